# Optimizing a Trainium2 kernel written in Bass

```python
import jax
import jax.numpy as jnp
from jax import lax
import numpy as np

D_MODEL = 4096
BATCH = 2
SEQ = 4096
DEPTH = 2

CTX_LEN = 256
GRID_W = 64
RMS_EPS = 1e-6

NA_HEADS = 8
NA_DIM = 128
NA_W = NA_HEADS * NA_DIM
NA_WIN_R = 8
NA_WIN_C = 16
MLA_HEADS = 12
MLA_Q_LORA = 1024
MLA_KV_LORA = 512
MLA_NOPE = 128
MLA_ROPE = 64
MLA_QK = MLA_NOPE + MLA_ROPE
MLA_V = 128
MLA_W = MLA_HEADS * MLA_V
ROPE_BASE = 10000.0
Q_BLOCK = 128
GLA_HEADS = 6
GLA_DK = 128
GLA_DV = 256
GLA_KW = GLA_HEADS * GLA_DK
GLA_W = GLA_HEADS * GLA_DV
GLA_GATE_RANK = 16
GLA_TAU = 16.0
GLA_CHUNK = 64
MIX_W = NA_W + MLA_W + GLA_W
IN_SPLITS = (NA_W, NA_W, NA_W, MLA_Q_LORA, MLA_KV_LORA, MLA_ROPE, GLA_KW, GLA_KW, GLA_W, GLA_W, GLA_GATE_RANK, GLA_GATE_RANK)
IN_W = 3 * NA_W + MLA_Q_LORA + MLA_KV_LORA + MLA_ROPE + 2 * GLA_KW + 2 * GLA_W + 2 * GLA_GATE_RANK
MOE_EXPERTS = 16
MOE_GROUPS = 4
MOE_PER_GROUP = MOE_EXPERTS // MOE_GROUPS
MOE_TOP_K = 2
MOE_FF = 1024
MOE_BLOCK = 256

kernel_name = 'hybrid_na_mla_gla_grouped_moe_dit'


def rms_norm(a, g):
    af = a.astype(jnp.float32)
    y = af * lax.rsqrt(jnp.mean(af * af, axis=-1, keepdims=True) + RMS_EPS)
    return (y * g.astype(jnp.float32)).astype(a.dtype)


def modulate(h, shift, scale):
    return h * (1 + scale) + shift


def split_heads(a, n_heads):
    return a.reshape(*a.shape[:-1], n_heads, a.shape[-1] // n_heads)


def merge_heads(a):
    return a.reshape(*a.shape[:-2], a.shape[-2] * a.shape[-1])


def rope_1d(a, pos):
    nf = a.shape[-1] // 2
    inv = ROPE_BASE ** (-jnp.arange(nf, dtype=jnp.float32) / nf)
    ang = pos.astype(jnp.float32)[:, None] * inv[None, :]
    cos = jnp.cos(ang)[None, :, None, :]
    sin = jnp.sin(ang)[None, :, None, :]
    a1 = a[..., :nf].astype(jnp.float32)
    a2 = a[..., nf:].astype(jnp.float32)
    return jnp.concatenate([a1 * cos - a2 * sin, a1 * sin + a2 * cos], axis=-1).astype(a.dtype)


def rope_tail(a, pos_row, pos_col):
    half = MLA_ROPE // 2
    rope = a[..., MLA_NOPE:]
    return jnp.concatenate([a[..., :MLA_NOPE], rope_1d(rope[..., :half], pos_row), rope_1d(rope[..., half:], pos_col)], axis=-1)


def dense_attention(q, k, v, scale):
    s = jnp.einsum('bqhd,bkhd->bhqk', q, k).astype(jnp.float32) * scale
    p = jax.nn.softmax(s, axis=-1).astype(v.dtype)
    return jnp.einsum('bhqk,bkhd->bqhd', p, v)


def blocked_attention(q, k, v, scale):
    B, S, H, dq = q.shape
    qb = jnp.moveaxis(q.reshape(B, S // Q_BLOCK, Q_BLOCK, H, dq), 1, 0)
    ob = lax.map(lambda qq: dense_attention(qq, k, v, scale), qb)
    return jnp.moveaxis(ob, 0, 1).reshape(B, S, H * v.shape[-1])


def neighbourhood_attention(q, k, v, k_ctx, v_ctx, rpb):
    B, S, H, Dh = q.shape
    rows = S // GRID_W
    wr = min(NA_WIN_R, rows)
    n_cb = GRID_W // NA_WIN_C
    kcw = 2 * NA_WIN_C
    qg = q.reshape(B, rows, GRID_W, H, Dh)
    kg = k.reshape(B, rows, GRID_W, H, Dh)
    vg = v.reshape(B, rows, GRID_W, H, Dh)
    qcol = np.arange(GRID_W).reshape(n_cb, NA_WIN_C)
    cb0 = np.clip(np.arange(n_cb) * NA_WIN_C - NA_WIN_C // 2, 0, GRID_W - kcw)
    kcol = cb0[:, None] + np.arange(kcw)
    ws = np.clip(qcol - NA_WIN_C // 2, 0, GRID_W - NA_WIN_C)
    col_valid = (kcol[:, None, :] >= ws[:, :, None]) & (kcol[:, None, :] < ws[:, :, None] + NA_WIN_C)
    dcol = np.clip(kcol[:, None, :] - qcol[:, :, None], -(NA_WIN_C - 1), NA_WIN_C - 1) + NA_WIN_C - 1
    rpb_c = rpb[:, :, dcol]
    scale = Dh ** -0.5
    n_loc = wr * kcw

    def row_block(r):
        r0 = jnp.clip(r - wr // 2, 0, rows - wr)
        k_r = lax.dynamic_slice_in_dim(kg, r0, wr, axis=1)[:, :, kcol]
        v_r = lax.dynamic_slice_in_dim(vg, r0, wr, axis=1)[:, :, kcol]
        q_r = lax.dynamic_index_in_dim(qg, r, axis=1, keepdims=False).reshape(B, n_cb, NA_WIN_C, H, Dh)
        s_loc = jnp.einsum('bjqhd,bwjkhd->bhjqwk', q_r, k_r).astype(jnp.float32) * scale
        dr = r0 + jnp.arange(wr) - r + (NA_WIN_R - 1)
        bias = jnp.take(rpb_c, dr, axis=1).transpose(0, 2, 3, 1, 4).astype(jnp.float32)
        s_loc = jnp.where(col_valid[:, :, None, :], s_loc + bias, -jnp.inf)
        s_ctx = jnp.einsum('bjqhd,blhd->bhjql', q_r, k_ctx).astype(jnp.float32) * scale
        s = jnp.concatenate([s_loc.reshape(B, H, n_cb, NA_WIN_C, n_loc), s_ctx], axis=-1)
        p = jax.nn.softmax(s, axis=-1).astype(v.dtype)
        p_loc = p[..., :n_loc].reshape(B, H, n_cb, NA_WIN_C, wr, kcw)
        o = jnp.einsum('bhjqwk,bwjkhd->bjqhd', p_loc, v_r) + jnp.einsum('bhjql,blhd->bjqhd', p[..., n_loc:], v_ctx)
        return o.reshape(B, GRID_W, H, Dh)

    out = lax.map(row_block, jnp.arange(rows, dtype=jnp.int32))
    return jnp.moveaxis(out, 0, 1).reshape(B, S, H * Dh)


def mla_q(cq, qa_g, w_uq, qn_g):
    q = split_heads(rms_norm(cq, qa_g) @ w_uq, MLA_HEADS)
    return rms_norm(q, qn_g)


def mla_kv(ckv, k_rope, kva_g, w_ukv, kn_g):
    kv = split_heads(rms_norm(ckv, kva_g) @ w_ukv, MLA_HEADS)
    kr = jnp.broadcast_to(k_rope[:, :, None, :], k_rope.shape[:2] + (MLA_HEADS, MLA_ROPE))
    k = rms_norm(jnp.concatenate([kv[..., :MLA_NOPE], kr], axis=-1), kn_g)
    return k, kv[..., MLA_NOPE:]


def gla_log_decay(a_low, w2, b):
    z = (a_low @ w2 + b).astype(jnp.float32)
    return split_heads(jax.nn.log_sigmoid(z) / GLA_TAU, GLA_HEADS)


def gla_scan(q, k, v, log_a, s0):
    B, T, H, Dk = q.shape
    Dv = v.shape[-1]
    n = T // GLA_CHUNK

    def chunks(a):
        return a.reshape(B, n, GLA_CHUNK, H, a.shape[-1]).transpose(1, 0, 3, 2, 4).astype(jnp.float32)

    causal = jnp.tril(jnp.ones((GLA_CHUNK, GLA_CHUNK), dtype=bool))

    def step(state, inp):
        qc, kc, vc, gc = inp
        b = jnp.cumsum(gc, axis=2)
        rel = jnp.where(causal[:, :, None], b[:, :, :, None, :] - b[:, :, None, :, :], -jnp.inf)
        att = jnp.einsum('bhid,bhjd,bhijd->bhij', qc, kc, jnp.exp(rel))
        out = jnp.einsum('bhid,bhde->bhie', qc * jnp.exp(b), state) + jnp.einsum('bhij,bhje->bhie', att, vc)
        b_end = b[:, :, -1, :]
        state = jnp.exp(b_end)[..., None] * state + jnp.einsum('bhcd,bhce->bhde', kc * jnp.exp(b_end[:, :, None, :] - b), vc)
        return state, out

    s_fin, o = lax.scan(step, s0, (chunks(q), chunks(k), chunks(v), chunks(log_a)))
    o = o.transpose(1, 0, 3, 2, 4).reshape(B, T, H, Dv)
    return o.astype(v.dtype), s_fin


def gla_bidirectional(q, k, v, a_f, a_b, q_ctx, k_ctx, v_ctx, a_f_ctx, a_b_ctx, w_f, b_f, w_b, b_b):
    def prep(q_, k_, v_):
        return split_heads(q_, GLA_HEADS) * GLA_DK ** -0.5, split_heads(k_, GLA_HEADS), split_heads(v_, GLA_HEADS)

    ql, kl, vl = prep(q, k, v)
    qc, kc, vc = prep(q_ctx, k_ctx, v_ctx)
    s0 = jnp.zeros((q.shape[0], GLA_HEADS, GLA_DK, GLA_DV), jnp.float32)
    flip = lambda a: jnp.flip(a, axis=1)
    o_cf, s_cf = gla_scan(qc, kc, vc, gla_log_decay(a_f_ctx, w_f, b_f), s0)
    o_lf, _ = gla_scan(ql, kl, vl, gla_log_decay(a_f, w_f, b_f), s_cf)
    o_cb, s_cb = gla_scan(flip(qc), flip(kc), flip(vc), flip(gla_log_decay(a_b_ctx, w_b, b_b)), s0)
    o_lb, _ = gla_scan(flip(ql), flip(kl), flip(vl), flip(gla_log_decay(a_b, w_b, b_b)), s_cb)
    return o_lf + flip(o_lb), o_cf + flip(o_cb)


def gla_output(o, gate, on_g):
    B, T = o.shape[:2]
    o = rms_norm(o, on_g) * jax.nn.silu(split_heads(gate, GLA_HEADS))
    return o.reshape(B, T, GLA_W)


def token_mixing(hl, hc, w_in, w_out, na_qn, na_kn, na_rpb, mla_qa_g, mla_kva_g, mla_w_uq, mla_w_ukv,
                 mla_qn, mla_kn, gla_wf, gla_bf, gla_wb, gla_bb, gla_on, pos_row, pos_col, need_ctx):
    split_at = np.cumsum(IN_SPLITS)[:-1].tolist()
    (na_q, na_k, na_v, mla_cq, mla_ckv, mla_kr, gla_q, gla_k, gla_v, gla_g, gla_af, gla_ab) = jnp.split(hl @ w_in, split_at, axis=-1)
    (na_q_ctx, na_k_ctx, na_v_ctx, mla_cq_ctx, mla_ckv_ctx, mla_kr_ctx, gla_q_ctx, gla_k_ctx, gla_v_ctx,
     gla_g_ctx, gla_af_ctx, gla_ab_ctx) = jnp.split(hc @ w_in, split_at, axis=-1)
    ka_ctx = rms_norm(split_heads(na_k_ctx, NA_HEADS), na_kn)
    va_ctx = split_heads(na_v_ctx, NA_HEADS)
    oa = neighbourhood_attention(rms_norm(split_heads(na_q, NA_HEADS), na_qn),
                                 rms_norm(split_heads(na_k, NA_HEADS), na_kn),
                                 split_heads(na_v, NA_HEADS), ka_ctx, va_ctx, na_rpb)
    kb, vb = mla_kv(mla_ckv, mla_kr, mla_kva_g, mla_w_ukv, mla_kn)
    kb = rope_tail(kb, pos_row, pos_col)
    qb = rope_tail(mla_q(mla_cq, mla_qa_g, mla_w_uq, mla_qn), pos_row, pos_col)
    kb_ctx, vb_ctx = mla_kv(mla_ckv_ctx, mla_kr_ctx, mla_kva_g, mla_w_ukv, mla_kn)
    ob = blocked_attention(qb, jnp.concatenate([kb_ctx, kb], axis=1), jnp.concatenate([vb_ctx, vb], axis=1), MLA_QK ** -0.5)
    oc, oc_ctx = gla_bidirectional(gla_q, gla_k, gla_v, gla_af, gla_ab, gla_q_ctx, gla_k_ctx, gla_v_ctx,
                                   gla_af_ctx, gla_ab_ctx, gla_wf, gla_bf, gla_wb, gla_bb)
    yl = jnp.concatenate([oa, ob, gla_output(oc, gla_g, gla_on)], axis=-1) @ w_out
    if not need_ctx:
        return yl, None
    qa_ctx = rms_norm(split_heads(na_q_ctx, NA_HEADS), na_qn)
    oa_ctx = merge_heads(dense_attention(qa_ctx, ka_ctx, va_ctx, NA_DIM ** -0.5))
    qb_ctx = mla_q(mla_cq_ctx, mla_qa_g, mla_w_uq, mla_qn)
    ob_ctx = merge_heads(dense_attention(qb_ctx, kb_ctx, vb_ctx, MLA_QK ** -0.5))
    yc = jnp.concatenate([oa_ctx, ob_ctx, gla_output(oc_ctx, gla_g_ctx, gla_on)], axis=-1) @ w_out
    return yl, yc


def group_limited_route(h, w_router, router_bias):
    N = h.shape[0]
    scores = jax.nn.sigmoid((h @ w_router).astype(jnp.float32))
    grouped = (scores + router_bias.astype(jnp.float32)).reshape(N, MOE_GROUPS, MOE_PER_GROUP)
    group_score = jnp.sum(lax.top_k(grouped, 2)[0], axis=-1)
    grp = jnp.argmax(group_score, axis=-1).astype(jnp.int32)
    in_group = jnp.take_along_axis(grouped, grp[:, None, None], axis=1)[:, 0]
    _, local = lax.top_k(in_group, MOE_TOP_K)
    expert_idx = grp[:, None] * MOE_PER_GROUP + local.astype(jnp.int32)
    w = jnp.take_along_axis(scores, expert_idx, axis=1)
    w = w / jnp.sum(w, axis=-1, keepdims=True)
    return expert_idx, w.astype(h.dtype)


def moe_ffn(h, w_router, router_bias, w1, w3, w2):
    N, D = h.shape
    E = w1.shape[0]
    expert_idx, expert_w = group_limited_route(h, w_router, router_bias)
    NK = N * MOE_TOP_K
    flat_e = expert_idx.reshape(NK)
    order = jnp.argsort(flat_e)
    sorted_e = flat_e[order]
    counts = jnp.bincount(flat_e, length=E).astype(jnp.int32)
    padded = (counts + MOE_BLOCK - 1) // MOE_BLOCK * MOE_BLOCK
    pad_end = jnp.cumsum(padded)
    pad_start = pad_end - padded
    seg_start = jnp.cumsum(counts) - counts
    dest = pad_start[sorted_e] + jnp.arange(NK, dtype=jnp.int32) - seg_start[sorted_e]
    n_blocks = (NK + E * (MOE_BLOCK - 1)) // MOE_BLOCK
    P = n_blocks * MOE_BLOCK
    row_token = jnp.full((P,), N, jnp.int32).at[dest].set((order // MOE_TOP_K).astype(jnp.int32))
    row_gate = jnp.zeros((P,), h.dtype).at[dest].set(expert_w.reshape(NK)[order])
    block_expert = jnp.minimum(jnp.searchsorted(pad_end, jnp.arange(n_blocks, dtype=jnp.int32) * MOE_BLOCK, side='right'), E - 1)
    h_pad = jnp.concatenate([h, jnp.zeros((1, D), h.dtype)], axis=0)
    xb = h_pad[row_token].reshape(n_blocks, MOE_BLOCK, D)

    def expert_block(args):
        xblk, e = args
        return (jax.nn.silu(xblk @ w1[e]) * (xblk @ w3[e])) @ w2[e]

    yb = lax.map(expert_block, (xb, block_expert)).reshape(P, D)
    return jax.ops.segment_sum(yb * row_gate[:, None], row_token, num_segments=N + 1)[:N]


def setup_inputs(seed: int = 0) -> dict:
    key = jax.random.key(seed)
    ks = iter(jax.random.split(key, 32))
    f32 = jnp.float32
    nrm = lambda shape, s: jax.random.normal(next(ks), shape, f32) * s
    gain = lambda shape: 1.0 + nrm(shape, 0.02)
    D = D_MODEL
    return {
        'x': nrm((BATCH, SEQ, D), 1.0),
        'c': nrm((BATCH, D), 1.0),
        'ctx': nrm((BATCH, CTX_LEN, D), 1.0),
        'c_ctx': nrm((D,), 1.0),
        'w_ada': nrm((DEPTH, D, 6 * D), 0.5 * D ** -0.5),
        'b_ada': nrm((DEPTH, 6 * D), 0.02),
        'norm1': gain((DEPTH, D)),
        'norm2': gain((DEPTH, D)),
        'w_in': nrm((DEPTH, D, IN_W), D ** -0.5),
        'w_out': nrm((DEPTH, MIX_W, D), MIX_W ** -0.5),
        'na_q_norm': gain((DEPTH, NA_DIM)),
        'na_k_norm': gain((DEPTH, NA_DIM)),
        'na_rpb': nrm((DEPTH, NA_HEADS, 2 * NA_WIN_R - 1, 2 * NA_WIN_C - 1), 0.2),
        'mla_qa_norm': gain((DEPTH, MLA_Q_LORA)),
        'mla_kva_norm': gain((DEPTH, MLA_KV_LORA)),
        'mla_w_uq': nrm((DEPTH, MLA_Q_LORA, MLA_HEADS * MLA_QK), MLA_Q_LORA ** -0.5),
        'mla_w_ukv': nrm((DEPTH, MLA_KV_LORA, MLA_HEADS * (MLA_NOPE + MLA_V)), MLA_KV_LORA ** -0.5),
        'mla_q_norm': gain((DEPTH, MLA_QK)),
        'mla_k_norm': gain((DEPTH, MLA_QK)),
        'gla_w_gate_f': nrm((DEPTH, GLA_GATE_RANK, GLA_KW), GLA_GATE_RANK ** -0.5),
        'gla_b_gate_f': nrm((DEPTH, GLA_KW), 0.1),
        'gla_w_gate_b': nrm((DEPTH, GLA_GATE_RANK, GLA_KW), GLA_GATE_RANK ** -0.5),
        'gla_b_gate_b': nrm((DEPTH, GLA_KW), 0.1),
        'gla_out_norm': gain((DEPTH, GLA_DV)),
        'w_router': nrm((D, MOE_EXPERTS), D ** -0.5),
        'router_bias': nrm((MOE_EXPERTS,), 0.01),
        'moe_w1': nrm((DEPTH, MOE_EXPERTS, D, MOE_FF), D ** -0.5),
        'moe_w3': nrm((DEPTH, MOE_EXPERTS, D, MOE_FF), D ** -0.5),
        'moe_w2': nrm((DEPTH, MOE_EXPERTS, MOE_FF, D), MOE_FF ** -0.5),
    }


def reference(x, c, ctx, c_ctx, w_ada, b_ada, norm1, norm2, w_in, w_out,
              na_q_norm, na_k_norm, na_rpb, mla_qa_norm, mla_kva_norm, mla_w_uq, mla_w_ukv,
              mla_q_norm, mla_k_norm, gla_w_gate_f, gla_b_gate_f, gla_w_gate_b, gla_b_gate_b,
              gla_out_norm, w_router, router_bias, moe_w1, moe_w3, moe_w2):
    B, S, D = x.shape
    L = ctx.shape[1]
    t = jnp.arange(S, dtype=jnp.int32)
    pos_row, pos_col = t // GRID_W, t % GRID_W
    silu_c = jax.nn.silu(c)
    silu_cc = jax.nn.silu(c_ctx)
    xl, xc = x, ctx
    for l in range(DEPTH):
        need_ctx = l < DEPTH - 1
        mod_l = silu_c @ w_ada[l] + b_ada[l]
        mod_c = silu_cc @ w_ada[l] + b_ada[l]
        sh1, sc1, g1, sh2, sc2, g2 = jnp.split(mod_l[:, None, :], 6, axis=-1)
        csh1, csc1, cg1, csh2, csc2, cg2 = jnp.split(mod_c, 6)
        hl = modulate(rms_norm(xl, norm1[l]), sh1, sc1)
        hc = modulate(rms_norm(xc, norm1[l]), csh1, csc1)
        yl, yc = token_mixing(hl, hc, w_in[l], w_out[l], na_q_norm[l], na_k_norm[l], na_rpb[l],
                              mla_qa_norm[l], mla_kva_norm[l], mla_w_uq[l], mla_w_ukv[l],
                              mla_q_norm[l], mla_k_norm[l], gla_w_gate_f[l], gla_b_gate_f[l],
                              gla_w_gate_b[l], gla_b_gate_b[l], gla_out_norm[l], pos_row, pos_col, need_ctx)
        xl = xl + g1 * yl
        hl = modulate(rms_norm(xl, norm2[l]), sh2, sc2).reshape(B * S, D)
        if need_ctx:
            xc = xc + cg1 * yc
            hc = modulate(rms_norm(xc, norm2[l]), csh2, csc2).reshape(B * L, D)
            h = jnp.concatenate([hl, hc], axis=0)
        else:
            h = hl
        y = moe_ffn(h, w_router, router_bias, moe_w1[l], moe_w3[l], moe_w2[l])
        xl = xl + g2 * y[:B * S].reshape(B, S, D)
        if need_ctx:
            xc = xc + cg2 * y[B * S:].reshape(B, L, D)
    return xl
```

```python
import numpy as np
from contextlib import ExitStack
import concourse.bass as bass
import concourse.mybir as mybir
from concourse.bass_utils import run_bass_kernel_spmd

F32 = mybir.dt.float32
BF16 = mybir.dt.bfloat16
AF = mybir.ActivationFunctionType
ALU = mybir.AluOpType
AX = mybir.AxisListType

COMPUTE = ("pe", "act", "dve", "pool")

D = 4096
SEQ = 4096
CTX = 256
T = SEQ + CTX
NT = T // 128
DEPTH = 2
EPS = 1e-6
IN_W = 9312
O_NAQ, O_NAK, O_NAV = 0, 1024, 2048
O_CQ, O_CKV, O_KR = 3072, 4096, 4608
O_GQ, O_GK, O_GV, O_GG, O_AF, O_AB = 4672, 5440, 6208, 7744, 9280, 9296
NEG = -30000.0
import os
RDBG = int(os.environ.get('RDBG', '0'))
RSKIP = os.environ.get('RSKIP', '')


class TT:
    def __init__(self, k, h, name):
        self.k = k
        self.h = h
        self.name = name
        self.w = None
        self.r = []
        self.dslot = None
        self.psum = False

    def __getitem__(self, idx):
        return V(self, self.h[idx])


class V:
    def __init__(self, t, ap):
        self.t = t
        self.ap = ap

    def __getitem__(self, idx):
        return V(self.t, self.ap[idx])

    def rearrange(self, *a, **kw):
        return V(self.t, self.ap.rearrange(*a, **kw))

    def bitcast(self, dt):
        return V(self.t, self.ap.bitcast(dt))

    def pbc(self, n):
        return V(self.t, self.ap.partition_broadcast(n))

    def bc(self, shape):
        return V(self.t, self.ap.to_broadcast(list(shape)))


class K:
    def __init__(self):
        self.nc = bass.Bass("TRN2", target_bir_lowering=False)
        nc = self.nc
        self.E = dict(pe=nc.tensor, act=nc.scalar, dve=nc.vector, pool=nc.gpsimd, sp=nc.sync)
        self.sems = {}
        self.cnt = {}
        for e in COMPUTE:
            self.sems[e] = nc.alloc_semaphore("c_" + e)
            self.cnt[e] = 0
        self.waited = {e: {} for e in self.E}
        self.nid = 0
        self.ninst = 0
        self.dpool = []
        self.stack = None
        self.phase_tiles = []
        self.ps = [TT(self, nc.alloc_psum_tensor(f"psb{i}", [128, 512], F32), f"psb{i}") for i in range(8)]
        self.rr = 0
        for t in self.ps:
            t.psum = True

    def begin(self):
        self.stack = ExitStack()
        self.phase_tiles = []

    def end(self):
        self.barrier()
        for t in self.phase_tiles:
            if t.dslot is not None:
                self.dpool.append(t.dslot)
                t.dslot = None
        self.stack.close()
        self.stack = None

    def sb(self, shape, dt=F32, name=None):
        self.nid += 1
        name = (name or "sb") + str(self.nid)
        h = self.stack.enter_context(self.nc.sbuf_tensor(name, list(shape), dt))
        t = TT(self, h, name)
        self.phase_tiles.append(t)
        return t

    def dram(self, name, shape, dt=F32, kind="Internal"):
        h = self.nc.dram_tensor(name, list(shape), dt, kind=kind)
        return TT(self, h, name)

    def _slot(self, t):
        if t.dslot is None:
            if self.dpool:
                t.dslot = self.dpool.pop()
            else:
                self.nid += 1
                key = "d%d" % self.nid
                self.sems[key] = self.nc.alloc_semaphore(key)
                self.cnt[key] = 0
                t.dslot = key
        return t.dslot

    def _wait(self, eng, dep):
        if dep is None:
            return
        key, val = dep
        if eng == "pe" and key == "pe":
            return
        if self.waited[eng].get(key, 0) >= val:
            return
        self.waited[eng][key] = val
        self.E[eng].wait_ge(self.sems[key], val)

    def _deps(self, eng, reads, writes):
        for v in reads:
            if v is not None:
                self._wait(eng, v.t.w)
                if v.t.psum:
                    for d in v.t.r:
                        if d[0] != eng:
                            self._wait(eng, d)
        for v in writes:
            t = v.t
            self._wait(eng, t.w)
            for d in t.r:
                self._wait(eng, d)

    def _done(self, dep, reads, writes):
        for v in reads:
            if v is None:
                continue
            r = v.t.r
            r.append(dep)
            if len(r) > 16:
                best = {}
                for kk, vv in r:
                    best[kk] = max(best.get(kk, 0), vv)
                v.t.r = list(best.items())
        for v in writes:
            v.t.w = dep
            v.t.r = []

    def op(self, eng, fn, reads, writes, inc=True):
        self._deps(eng, reads, writes)
        ins = fn()
        self.ninst += 1
        if inc:
            self.cnt[eng] += 1
            ins.then_inc(self.sems[eng], 1)
            self._done((eng, self.cnt[eng]), reads, writes)
        else:
            self._done((eng, self.cnt[eng] + 1), reads, writes)
        return ins

    def dma(self, out, in_, q=None, **kw):
        if q is None:
            sb_out = out.t in self.phase_tiles or out.t in self.ps
            q = "sp" if sb_out else "act"
            if out.ap.dtype != in_.ap.dtype:
                q = "pool"
        owner = out.t
        key = self._slot(owner)
        self._deps(q, [in_], [out])
        ins = self.E[q].dma_start(out=out.ap, in_=in_.ap, **kw)
        self.cnt[key] += 16
        ins.then_inc(self.sems[key], 16)
        self._done((key, self.cnt[key]), [in_], [out])
        self.ninst += 1
        return ins

    def barrier(self):
        for eng in self.E:
            for key, c in self.cnt.items():
                if c > 0:
                    self._wait(eng, (key, c))

    def matmul(self, out, lhsT, rhs, start=True, stop=True):
        return self.op("pe", lambda: self.nc.tensor.matmul(out.ap, lhsT.ap, rhs.ap, start=start, stop=stop),
                       [lhsT, rhs], [out], inc=stop)

    def transpose(self, out, in_, ident, inc=True):
        return self.op("pe", lambda: self.nc.tensor.transpose(out.ap, in_.ap, ident.ap), [in_, ident], [out], inc=inc)

    def act(self, out, in_, func, bias=None, scale=None, accum=None):
        kw = {}
        reads = [in_]
        writes = [out]
        if bias is not None:
            if isinstance(bias, V):
                kw["bias"] = bias.ap
                reads.append(bias)
            else:
                kw["bias"] = bias
        if scale is not None:
            if isinstance(scale, V):
                kw["scale"] = scale.ap
                reads.append(scale)
            else:
                kw["scale"] = scale
        if accum is not None:
            kw["accum_out"] = accum.ap
            writes.append(accum)
        return self.op("act", lambda: self.nc.scalar.activation(out.ap, in_.ap, func, **kw), reads, writes)

    def copy(self, out, in_, eng=None):
        if eng is None:
            self.rr ^= 1
            eng = "dve" if self.rr else "act"
        if eng == "act":
            return self.act(out, in_, AF.Copy)
        return self.op(eng, lambda: self.E[eng].tensor_copy(out.ap, in_.ap), [in_], [out])

    def tt(self, out, a, b, op, eng="dve"):
        return self.op(eng, lambda: self.E[eng].tensor_tensor(out.ap, a.ap, b.ap, op), [a, b], [out])

    def ts(self, out, a, s1, op0, s2=None, op1=None, accum=None, eng="dve"):
        reads = [a]
        writes = [out]
        s1a = s1.ap if isinstance(s1, V) else s1
        s2a = s2.ap if isinstance(s2, V) else s2
        if isinstance(s1, V):
            reads.append(s1)
        if isinstance(s2, V):
            reads.append(s2)
        kw = {}
        if op1 is not None:
            kw["op1"] = op1
        if accum is not None:
            kw["accum_out"] = accum.ap
            writes.append(accum)
        return self.op(eng, lambda: self.E[eng].tensor_scalar(out.ap, a.ap, s1a, s2a, op0, **kw), reads, writes)

    def stt(self, out, a, s, b, op0, op1, eng="dve"):
        reads = [a, b]
        sa = s.ap if isinstance(s, V) else s
        if isinstance(s, V):
            reads.append(s)
        return self.op(eng, lambda: self.E[eng].scalar_tensor_tensor(out.ap, a.ap, sa, b.ap, op0, op1),
                       reads, [out])

    def reduce(self, out, in_, op, axis=AX.X, eng="dve"):
        return self.op(eng, lambda: self.E[eng].tensor_reduce(out.ap, in_.ap, axis, op), [in_], [out])

    def recip(self, out, in_, eng="dve"):
        return self.op(eng, lambda: self.E[eng].reciprocal(out.ap, in_.ap), [in_], [out])

    def memset(self, out, val, eng="dve"):
        return self.op(eng, lambda: self.E[eng].memset(out.ap, val), [], [out])

    def rstd(self, out, ss, n):
        self.ts(out, ss, 1.0 / n, ALU.mult, EPS, ALU.add)
        self.act(out, out, AF.Sqrt)
        self.recip(out, out)


def ph_ada(k, I, MOD):
    k.begin()
    cT = k.sb([128, 32, 2], F32)
    k.dma(cT[:], I["cT"][:])
    sT = k.sb([128, 32, 2], BF16)
    k.act(sT[:], cT[:], AF.Silu)
    wt = [k.sb([128, 8, 512], BF16) for _ in range(4)]
    bt = [k.sb([2, 512], F32) for _ in range(2)]
    ot = [k.sb([2, 512], F32) for _ in range(2)]
    n = 0
    for l in range(DEPTH):
        for nt in range(48):
            ps = k.ps[nt % 2]
            for kq in range(4):
                w = wt[n % 4]
                n += 1
                src = I["w_ada"][l, kq * 1024:(kq + 1) * 1024, nt * 512:(nt + 1) * 512]
                k.dma(w[:], src.rearrange("(c p) n -> p c n", p=128))
                for c in range(8):
                    kc = kq * 8 + c
                    k.matmul(ps[0:2, :], sT[:, kc, :], w[:, c, :], start=(kc == 0), stop=(kc == 31))
            b = bt[nt % 2]
            o = ot[nt % 2]
            k.dma(b[:], I["b_ada"][l:l + 1, nt * 512:(nt + 1) * 512].pbc(2) if False else
                  I["b_ada"][l, nt * 512:(nt + 1) * 512].pbc(2))
            k.tt(o[:], ps[0:2, :], b[:], ALU.add)
            k.dma(MOD[l, :, nt * 512:(nt + 1) * 512], o[:])
    k.end()


def ph_norm(k, I, l, which, X, MOD, HT, tiles, router=None):
    k.begin()
    gain = I["norm1" if which == 0 else "norm2"]
    sh_i, sc_i = (0, 1) if which == 0 else (3, 4)
    ident = k.sb([128, 128], F32)
    k.dma(ident[:], I["ident"][:])
    tmps = [k.sb([128, D], F32) for _ in range(2)]
    hfs = [k.sb([128, D], F32) for _ in range(2)]
    tmp, hf = tmps[0], hfs[0]
    A = [k.sb([128, D], F32) for _ in range(2)]
    Bv = [k.sb([128, D], F32) for _ in range(2)]
    k.dma(hf[:], gain[l, :].pbc(128))
    for r in range(2):
        k.dma(tmp[:], MOD[l, r, sc_i * D:(sc_i + 1) * D].pbc(128))
        k.dma(Bv[r][:], MOD[l, r, sh_i * D:(sh_i + 1) * D].pbc(128))
        k.stt(A[r][:], tmp[:], 1.0, hf[:], ALU.add, ALU.mult)
    xb = [k.sb([128, D], F32) for _ in range(2)]
    hT = [k.sb([128, 32, 128], BF16) for _ in range(2)]
    ss = [k.sb([128, 1], F32) for _ in range(2)]
    if router is not None:
        wr = k.sb([128, 32, 16], F32)
        k.dma(wr[:], I["wrT"][:])
        rb = k.sb([128, 16], F32)
        k.dma(rb[:], I["router_bias"][:].pbc(128))
        hTf = [k.sb([128, 4, 128], F32) for _ in range(2)]
        sc = k.sb([128, 16], F32)
        lg = k.sb([128, 16], F32)
        bs = k.sb([128, 16], F32)
        m1 = k.sb([128, 4], F32)
        m2 = k.sb([128, 4], F32)
        t16 = k.sb([128, 16], F32)
        gs = k.sb([128, 4], F32)
        gm = k.sb([128, 1], F32)
        sel = k.sb([128, 16], F32)
        gt = [k.sb([128, 16], F32) for _ in range(2)]
        gTt = [k.sb([16, 128], F32) for _ in range(2)]
    for n, i in enumerate(tiles):
        r = 1 if i < 2 else 0
        x = xb[n % 2]
        s = ss[n % 2]
        h = hT[n % 2]
        tmp, hf = tmps[n % 2], hfs[n % 2]
        k.dma(x[:], X[i * 128:(i + 1) * 128, :])
        k.memset(s[:], 0.0)
        k.act(tmp[:], x[:], AF.Square, accum=s[:])
        k.rstd(s[:], s[:], D)
        k.stt(tmp[:], x[:], s[:, 0:1], A[r][:], ALU.mult, ALU.mult)
        k.tt(hf[:], tmp[:], Bv[r][:], ALU.add)
        psr = k.ps[7]
        for c4 in range(8):
            ps = k.ps[c4 % 4]
            for j in range(4):
                c = c4 * 4 + j
                k.transpose(ps[:, j * 128:(j + 1) * 128], hf[:, c * 128:(c + 1) * 128], ident[:], inc=(j == 3))
            ceng = "dve" if c4 % 2 else "act"
            k.copy(h[:, c4 * 4:(c4 + 1) * 4, :], ps[:].rearrange("p (j t) -> p j t", j=4), eng=ceng)
            if router is not None:
                hx = hTf[c4 % 2]
                if 'h' not in RSKIP:
                    k.copy(hx[:], ps[:].rearrange("p (j t) -> p j t", j=4), eng=ceng)
                for j in range(4):
                    c = c4 * 4 + j
                    if RDBG == 3:
                        continue
                    k.matmul(psr[:, c * 16:(c + 1) * 16], hx[:, j, :], wr[:, c, :], start=True, stop=True)
        k.dma(HT[:, :, i * 128:(i + 1) * 128].rearrange("c p t -> p c t"), h[:])
        if router is not None:
            g = gt[n % 2]
            if 'r' not in RSKIP:
                k.reduce(lg[:], psr[:, :].rearrange("p (c e) -> p e c", e=16), ALU.add)
            else:
                k.memset(lg[:], 0.5)
            if 's' not in RSKIP:
                k.act(sc[:], lg[:], AF.Sigmoid)
            else:
                k.copy(sc[:], lg[:], eng='dve')
            if RDBG in (1, 3):
                k.dma(router["GATE"][i * 128:(i + 1) * 128, :], sc[:])
                continue
            k.tt(bs[:], sc[:], rb[:], ALU.add)
            bs3 = bs[:].rearrange("p (g e) -> p g e", g=4)
            k.reduce(m1[:], bs3, ALU.max)
            k.tt(t16[:].rearrange("p (g e) -> p g e", g=4), bs3, m1[:].bc([128, 4, 4]) if False else
                 m1[:].rearrange("p (g o) -> p g o", o=1).bc([128, 4, 4]), ALU.is_equal)
            k.stt(t16[:], t16[:], -1e4, bs[:], ALU.mult, ALU.add)
            k.reduce(m2[:], t16[:].rearrange("p (g e) -> p g e", g=4), ALU.max)
            k.tt(gs[:], m1[:], m2[:], ALU.add)
            k.reduce(gm[:], gs[:], ALU.max)
            k.ts(gs[:], gs[:], gm[:, 0:1], ALU.is_equal)
            k.tt(sel[:].rearrange("p (g e) -> p g e", g=4), bs3,
                 m2[:].rearrange("p (g o) -> p g o", o=1).bc([128, 4, 4]), ALU.is_ge)
            k.tt(sel[:].rearrange("p (g e) -> p g e", g=4), sel[:].rearrange("p (g e) -> p g e", g=4),
                 gs[:].rearrange("p (g o) -> p g o", o=1).bc([128, 4, 4]), ALU.mult)
            k.tt(sel[:], sel[:], sc[:], ALU.mult)
            k.reduce(gm[:], sel[:], ALU.add)
            k.recip(gm[:], gm[:])
            k.ts(g[:], sel[:], gm[:, 0:1], ALU.mult)
            k.dma(router["GATE"][i * 128:(i + 1) * 128, :], g[:])
            if RDBG == 2:
                continue
            k.transpose(k.ps[6][0:16, 0:128], g[:], ident[:])
            gT = gTt[n % 2]
            k.copy(gT[:], k.ps[6][0:16, 0:128], eng="dve")
            k.dma(router["GATET"][:, i * 128:(i + 1) * 128], gT[:])
    k.end()


def gemm(k, AT, Kc, W, N, tiles, epilogue, slab=4, wdt=BF16, ng=512, setup=None, nat=2):
    k.begin()
    ectx = setup() if setup else None
    slabs = [tiles[s0:s0 + slab] for s0 in range(0, len(tiles), slab)]
    maxs = max(len(st) for st in slabs)
    at = [k.sb([128, Kc, maxs * 128], BF16) for _ in range(nat)]
    wt = [k.sb([128, Kc, ng], wdt) for _ in range(2)]
    nw = 0
    na = 0
    npb = 0
    for n0 in range(0, N, ng):
        nn = min(ng, N - n0)
        w = wt[nw % 2]
        nw += 1
        hc = max(1, Kc // 2)
        for c0 in range(0, Kc, hc):
            c1 = min(Kc, c0 + hc)
            k.dma(w[:, c0:c1, 0:nn], W[c0 * 128:c1 * 128, n0:n0 + nn].rearrange("(c p) n -> p c n", p=128))
        for st in slabs:
            assert st == list(range(st[0], st[0] + len(st)))
            ntok = len(st) * 128
            a = at[na % nat]
            na += 1
            k.dma(a[:, :, 0:ntok], AT[:, :, st[0] * 128:st[0] * 128 + ntok].rearrange("c p t -> p c t"))
            for j, i in enumerate(st):
                ps = k.ps[npb % 6]
                npb += 1
                for c in range(Kc):
                    k.matmul(ps[:, 0:nn], a[:, c, j * 128:(j + 1) * 128], w[:, c, 0:nn],
                             start=(c == 0), stop=(c == Kc - 1))
                epilogue(ectx, i, n0, nn, ps[:, 0:nn])
    k.end()


def ph_proj_in(k, I, l, HT, P, tiles):
    def setup():
        return [k.sb([128, 512], F32) for _ in range(3)]

    def ep(ob, i, n0, nn, ps):
        o = ob[(i + n0 // 512) % 3]
        k.copy(o[:, 0:nn], ps)
        k.dma(P[i * 128:(i + 1) * 128, n0:n0 + nn], o[:, 0:nn])

    gemm(k, HT, 32, I["w_in"][l], IN_W, tiles, ep, setup=setup, nat=3)


def load_ident_bf(k, I):
    ib = k.sb([128, 128], BF16)
    k.dma(ib[:], I["ident"][:])
    return ib


def head_rms(k, dst, src3, gain3, ss, tmp3, nh, dh, extra=None):
    k.tt(tmp3, src3, src3, ALU.mult)
    k.reduce(ss, tmp3, ALU.add)
    n = dh
    if extra is not None:
        k.ts(ss, ss, extra[0], ALU.add)
        n = extra[1]
    k.rstd(ss, ss, n)
    k.tt(tmp3, src3, ss.rearrange("p (h o) -> p h o", o=1).bc([128, nh, dh]), ALU.mult)
    k.tt(dst, tmp3, gain3, ALU.mult)


def ph_na_prep(k, I, l, P, NQT, NKT, tiles):
    k.begin()
    ib = load_ident_bf(k, I)
    Gq = k.sb([128, 8, 128], F32)
    Gk = k.sb([128, 8, 128], F32)
    for h in range(8):
        k.dma(Gq[:, h, :], I["na_q_norm"][l, :].pbc(128))
        k.dma(Gk[:, h, :], I["na_k_norm"][l, :].pbc(128))
    k.ts(Gq[:], Gq[:], 128.0 ** -0.5, ALU.mult)
    xin = [k.sb([128, 1024], F32) for _ in range(2)]
    tmp = k.sb([128, 1024], F32)
    xn = [k.sb([128, 1024], BF16) for _ in range(2)]
    ss = k.sb([128, 8], F32)
    xT = [k.sb([128, 8, 128], BF16) for _ in range(2)]
    n = 0
    for i in tiles:
        for (off, G, DST) in ((O_NAQ, Gq, NQT), (O_NAK, Gk, NKT)):
            x = xin[n % 2]
            y = xn[n % 2]
            o = xT[n % 2]
            k.dma(x[:], P[i * 128:(i + 1) * 128, off:off + 1024])
            x3 = x[:].rearrange("p (h d) -> p h d", h=8)
            head_rms(k, y[:].rearrange("p (h d) -> p h d", h=8), x3, G[:], ss[:],
                     tmp[:].rearrange("p (h d) -> p h d", h=8), 8, 128)
            ps = k.ps[n % 4].bitcast(BF16) if False else k.ps[n % 4][:].bitcast(BF16)
            for h in range(8):
                k.transpose(ps[:, h * 128:(h + 1) * 128], y[:, h * 128:(h + 1) * 128], ib[:], inc=(h == 7))
            k.copy(o[:], ps.rearrange("p (h t) -> p h t", h=8))
            k.dma(DST[:, :, i * 128:(i + 1) * 128].rearrange("h p t -> p h t"), o[:])
            n += 1
    k.end()


def ph_na_attn(k, I, l, P, NQT, NKT, OT, need_ctx):
    k.begin()
    ib = load_ident_bf(k, I)
    mask = k.sb([64, 64], F32)
    k.dma(mask[:], I["na_mask"][:])
    KT = k.sb([128, T], BF16)
    QT = k.sb([128, T], BF16)
    V64 = k.sb([64, 68, 129], BF16)
    bias = k.sb([64, 15, 64], F32)
    OTh = k.sb([128, T], BF16)
    Ssb = [k.sb([64, 512], F32) for _ in range(2)]
    PT = [k.sb([64, 768], BF16) for _ in range(3)]
    rs = [k.sb([64, 1], F32) for _ in range(2)]
    On = [k.sb([64, 128], BF16) for _ in range(3)]
    n = 0
    for h in range(8):
        k.dma(KT[:], NKT[h, :, :])
        k.dma(QT[:], NQT[h, :, :])
        k.dma(V64[:, :, 0:128], P[:, O_NAV + h * 128:O_NAV + (h + 1) * 128].rearrange("(r p) d -> p r d", p=64))
        k.memset(V64[:, :, 128:129], 1.0)
        k.dma(bias[:], I["na_bt"][l, h, :, :, :].rearrange("r k q -> k r q"))
        k.tt(bias[:], bias[:], mask[:].rearrange("k (o q) -> k o q", o=1).bc([64, 15, 64]), ALU.add)
        if not need_ctx:
            k.memset(OTh[:, 0:CTX], 0.0)
        blocks = [("lat", r) for r in range(64)] + ([("ctx", r) for r in range(4)] if need_ctx else [])

        def stage_a(kind, r, n_):
            psA = k.ps[n_ % 2]
            psB = k.ps[2 + n_ % 2]
            S = Ssb[n_ % 2]
            pt = PT[n_ % 3]
            if kind == "lat":
                tq = CTX + 64 * r
                r0 = min(max(r - 4, 0), 56)
                dr0 = r0 - r + 7
                for j in range(8):
                    tk = CTX + 64 * (r0 + j)
                    k.matmul(psA[0:64, j * 64:(j + 1) * 64], KT[:, tk:tk + 64], QT[:, tq:tq + 64])
                for j in range(4):
                    k.matmul(psB[0:64, j * 64:(j + 1) * 64], KT[:, 64 * j:64 * j + 64], QT[:, tq:tq + 64])
                k.tt(S[:], psA[0:64, :], bias[:, dr0:dr0 + 8, :].rearrange("k r q -> k (r q)"), ALU.add)
                k.act(pt[:, 0:512], S[:], AF.Exp)
                k.act(pt[:, 512:768], psB[0:64, 0:256], AF.Exp)
            else:
                tq = 64 * r
                for j in range(4):
                    k.matmul(psB[0:64, j * 64:(j + 1) * 64], KT[:, 64 * j:64 * j + 64], QT[:, tq:tq + 64])
                k.act(pt[:, 512:768], psB[0:64, 0:256], AF.Exp)

        def stage_b(kind, r, n_):
            psO = k.ps[4 + n_ % 2]
            pt = PT[n_ % 3]
            if kind == "lat":
                r0 = min(max(r - 4, 0), 56)
                for j in range(8):
                    k.matmul(psO[0:64, 0:129], pt[:, j * 64:(j + 1) * 64], V64[:, 4 + r0 + j, :],
                             start=(j == 0), stop=False)
                for j in range(4):
                    k.matmul(psO[0:64, 0:129], pt[:, 512 + j * 64:512 + (j + 1) * 64], V64[:, j, :],
                             start=False, stop=(j == 3))
            else:
                for j in range(4):
                    k.matmul(psO[0:64, 0:129], pt[:, 512 + j * 64:512 + (j + 1) * 64], V64[:, j, :],
                             start=(j == 0), stop=(j == 3))
            rr = rs[n_ % 2]
            on = On[n_ % 3]
            k.recip(rr[:], psO[0:64, 128:129])
            k.ts(on[:], psO[0:64, 0:128], rr[:, 0:1], ALU.mult)

        def stage_c(kind, r, n_):
            tq = CTX + 64 * r if kind == "lat" else 64 * r
            psT = k.ps[6 + n_ % 2][:].bitcast(BF16)
            on = On[n_ % 3]
            k.transpose(psT[:, 0:64], on[:], ib[0:64, 0:64])
            k.copy(OTh[:, tq:tq + 64], psT[:, 0:64])

        nb = len(blocks)
        for step in range(nb + 2):
            if step < nb:
                stage_a(blocks[step][0], blocks[step][1], n + step)
            if 1 <= step <= nb:
                stage_b(blocks[step - 1][0], blocks[step - 1][1], n + step - 1)
            if 2 <= step:
                stage_c(blocks[step - 2][0], blocks[step - 2][1], n + step - 2)
        n += nb
        k.dma(OT[h, :, :], OTh[:])
    k.end()


def host_consts(inputs):
    c = {}
    q = np.arange(64)
    ws = np.clip(q - 8, 0, 48)
    kk = np.arange(64)
    valid = (kk[:, None] >= ws[None, :]) & (kk[:, None] < ws[None, :] + 16)
    c["na_mask"] = np.where(valid, 0.0, NEG).astype(np.float32)
    dcol = np.clip(kk[:, None] - q[None, :], -15, 15) + 15
    c["na_bt"] = np.ascontiguousarray(inputs["na_rpb"][:, :, :, dcol])
    t = np.arange(SEQ)
    inv = (10000.0 ** (-np.arange(16, dtype=np.float32) / 16)).astype(np.float32)
    C = np.zeros((SEQ, 64), np.float32)
    S = np.zeros((SEQ, 64), np.float32)
    for a, pos in enumerate((t // 64, t % 64)):
        ang = pos.astype(np.float32)[:, None] * inv[None, :]
        C[:, a * 32:a * 32 + 16] = np.cos(ang)
        C[:, a * 32 + 16:a * 32 + 32] = np.cos(ang)
        S[:, a * 32:a * 32 + 16] = -np.sin(ang)
        S[:, a * 32 + 16:a * 32 + 32] = np.sin(ang)
    c["ropeC"] = C
    c["ropeS"] = S
    j = np.arange(64)[:, None]
    i = np.arange(64)[None, :]
    le = [(j <= i), (j >= i)]
    c["gl_L"] = np.stack([np.where(m, -1.0 / 16, 0.0) for m in le]).astype(np.float32)
    c["gl_U"] = np.stack([np.where(~m, -1.0 / 16, 0.0) for m in le]).astype(np.float32)
    c["gl_M"] = np.stack([np.where(m, 1.0, 0.0) for m in le]).astype(np.float32)
    sel = np.zeros((16, 16, 128), np.float32)
    for e in range(16):
        sel[e, e, :] = 1.0
    c["selT"] = sel
    c["ident"] = np.eye(128, dtype=np.float32)
    return c


def mla_scratch(k):
    return dict(CQT=k.dram("CQT", [8, 128, T], BF16), CKVT=k.dram("CKVT", [4, 128, T], BF16),
                Q2=k.dram("Q2", [T, 2304], F32), KV2=k.dram("KV2", [T, 3072], F32),
                MQT=k.dram("MQT", [12, 2, 128, T], BF16), MKT=k.dram("MKT", [12, 2, 128, T], BF16))


def ph_mla(k, I, l, P, S, OT, need_ctx):
    tiles = list(range(NT))
    k.begin()
    ib = load_ident_bf(k, I)
    Gq = k.sb([128, 1024], F32)
    Gk = k.sb([128, 512], F32)
    k.dma(Gq[:], I["mla_qa_norm"][l, :].pbc(128))
    k.dma(Gk[:], I["mla_kva_norm"][l, :].pbc(128))
    xin = [k.sb([128, 1536], F32) for _ in range(2)]
    tmp = k.sb([128, 1024], F32)
    y = [k.sb([128, 1536], BF16) for _ in range(2)]
    ss = k.sb([128, 2], F32)
    oT = [k.sb([128, 12, 128], BF16) for _ in range(2)]
    for n, i in enumerate(tiles):
        x = xin[n % 2]
        yy = y[n % 2]
        o = oT[n % 2]
        k.dma(x[:], P[i * 128:(i + 1) * 128, O_CQ:O_CQ + 1536])
        for (a, w, G, sidx) in ((0, 1024, Gq, 0), (1024, 512, Gk, 1)):
            k.memset(ss[:, sidx:sidx + 1], 0.0)
            k.act(tmp[:, 0:w], x[:, a:a + w], AF.Square, accum=ss[:, sidx:sidx + 1])
            k.rstd(ss[:, sidx:sidx + 1], ss[:, sidx:sidx + 1], w)
            k.stt(yy[:, a:a + w], x[:, a:a + w], ss[:, sidx:sidx + 1], G[:], ALU.mult, ALU.mult)
        for g in range(2):
            ps = k.ps[(2 * n + g) % 4][:].bitcast(BF16)
            cn = 8 if g == 0 else 4
            for c in range(cn):
                cc = g * 8 + c
                k.transpose(ps[:, c * 128:(c + 1) * 128], yy[:, cc * 128:(cc + 1) * 128], ib[:], inc=(c == cn - 1))
            k.copy(o[:, g * 8:g * 8 + cn, :], ps[:, 0:cn * 128].rearrange("p (c t) -> p c t", c=cn))
        k.dma(S["CQT"][:, :, i * 128:(i + 1) * 128].rearrange("c p t -> p c t"), o[:, 0:8, :])
        k.dma(S["CKVT"][:, :, i * 128:(i + 1) * 128].rearrange("c p t -> p c t"), o[:, 8:12, :])
    k.end()

    def setup():
        return [k.sb([128, 512], F32) for _ in range(3)]

    def mk_ep(DST):
        def ep(ob, i, n0, nn, ps):
            o = ob[(i + n0 // 512) % 3]
            k.copy(o[:, 0:nn], ps)
            k.dma(DST[i * 128:(i + 1) * 128, n0:n0 + nn], o[:, 0:nn])
        return ep
    gemm(k, S["CQT"], 8, I["mla_w_uq"][l], 2304, tiles, mk_ep(S["Q2"]), slab=9, setup=setup)
    gemm(k, S["CKVT"], 4, I["mla_w_ukv"][l], 3072, tiles, mk_ep(S["KV2"]), slab=9, setup=setup)

    k.begin()
    ib = load_ident_bf(k, I)
    Gq = k.sb([128, 12, 192], F32)
    Gkn = k.sb([128, 128], F32)
    Gkr = k.sb([128, 64], F32)
    for h in range(12):
        k.dma(Gq[:, h, :], I["mla_q_norm"][l, :].pbc(128))
    k.ts(Gq[:], Gq[:], 192.0 ** -0.5, ALU.mult)
    k.dma(Gkn[:], I["mla_k_norm"][l, 0:128].pbc(128))
    k.dma(Gkr[:], I["mla_k_norm"][l, 128:192].pbc(128))
    qin = [k.sb([128, 12, 192], F32) for _ in range(2)]
    kvin = [k.sb([128, 12, 256], F32) for _ in range(2)]
    krin = [k.sb([128, 64], F32) for _ in range(2)]
    rc = [k.sb([128, 64], F32) for _ in range(2)]
    rsn = [k.sb([128, 64], F32) for _ in range(2)]
    tq = k.sb([128, 12, 192], F32)
    qn = k.sb([128, 12, 192], F32)
    xsw = k.sb([128, 12, 64], F32)
    t1 = k.sb([128, 12, 64], F32)
    qb = k.sb([128, 12, 192], BF16)
    kn = k.sb([128, 12, 192], F32)
    kb = k.sb([128, 12, 192], BF16)
    krg = k.sb([128, 64], F32)
    ss = k.sb([128, 12], F32)
    skr = k.sb([128, 1], F32)
    oa = [k.sb([128, 12, 128], BF16) for _ in range(2)]
    ob_ = [k.sb([64, 12, 128], BF16) for _ in range(2)]

    def rope(x3, nh, n):
        x5 = x3.rearrange("p h (a b d) -> p h a b d", a=2, b=2)
        xs3 = xsw[:, 0:nh, :]
        xs5 = xs3.rearrange("p h (a b d) -> p h a b d", a=2, b=2)
        for a in range(2):
            k.copy(xs5[:, :, a, 0, :], x5[:, :, a, 1, :], eng="dve")
            k.copy(xs5[:, :, a, 1, :], x5[:, :, a, 0, :], eng="dve")
        cb = rc[n % 2][:].rearrange("p (o d) -> p o d", o=1).bc([128, nh, 64])
        sb_ = rsn[n % 2][:].rearrange("p (o d) -> p o d", o=1).bc([128, nh, 64])
        k.tt(t1[:, 0:nh, :], x3, cb, ALU.mult)
        k.tt(xs3, xs3, sb_, ALU.mult)
        k.tt(x3, t1[:, 0:nh, :], xs3, ALU.add)

    cnt = 0
    for n, i in enumerate(tiles):
        latent = i >= 2
        q = qin[n % 2]
        kv = kvin[n % 2]
        kr = krin[n % 2]
        k.dma(q[:].rearrange("p h d -> p (h d)"), S["Q2"][i * 128:(i + 1) * 128, :])
        k.dma(kv[:].rearrange("p h d -> p (h d)"), S["KV2"][i * 128:(i + 1) * 128, :])
        k.dma(kr[:], P[i * 128:(i + 1) * 128, O_KR:O_KR + 64])
        if latent:
            k.dma(rc[n % 2][:], I["ropeC"][(i - 2) * 128:(i - 1) * 128, :])
            k.dma(rsn[n % 2][:], I["ropeS"][(i - 2) * 128:(i - 1) * 128, :])
        head_rms(k, qn[:], q[:], Gq[:], ss[:], tq[:], 12, 192)
        if latent:
            rope(qn[:, :, 128:192], 12, n)
        k.copy(qb[:], qn[:], eng="act")
        k.memset(skr[:], 0.0)
        k.act(t1[:, 0, :], kr[:], AF.Square, accum=skr[:])
        head_rms(k, kn[:, :, 0:128], kv[:, :, 0:128], Gkn[:].rearrange("p (o d) -> p o d", o=1).bc([128, 12, 128]),
                 ss[:], tq[:, :, 0:128], 12, 128, extra=(skr[:, 0:1], 192))
        k.tt(krg[:], kr[:], Gkr[:], ALU.mult)
        if latent:
            rope(krg[:].rearrange("p (o d) -> p o d", o=1), 1, n)
        k.tt(kn[:, :, 128:192], krg[:].rearrange("p (o d) -> p o d", o=1).bc([128, 12, 64]),
             ss[:].rearrange("p (h o) -> p h o", o=1).bc([128, 12, 64]), ALU.mult)
        k.copy(kb[:], kn[:], eng="act")
        for (src, DST) in ((qb, S["MQT"]), (kb, S["MKT"])):
            a = oa[cnt % 2]
            b = ob_[cnt % 2]
            cnt += 1
            for g in range(2):
                hs = range(8) if g == 0 else range(8, 12)
                ps = k.ps[(2 * cnt + g) % 4][:].bitcast(BF16)
                for jj, h in enumerate(hs):
                    k.transpose(ps[:, jj * 128:(jj + 1) * 128], src[:, h, 0:128], ib[:], inc=(jj == len(hs) - 1))
                k.copy(a[:, hs[0]:hs[0] + len(hs), :], ps[:, 0:len(hs) * 128].rearrange("p (c t) -> p c t", c=len(hs)))
            for g in range(2):
                hs = range(8) if g == 0 else range(8, 12)
                ps = k.ps[4 + (2 * cnt + g) % 4][:].bitcast(BF16)
                for jj, h in enumerate(hs):
                    k.transpose(ps[0:64, jj * 128:(jj + 1) * 128], src[:, h, 128:192], ib[:], inc=(jj == len(hs) - 1))
                k.copy(b[:, hs[0]:hs[0] + len(hs), :], ps[0:64, 0:len(hs) * 128].rearrange("p (c t) -> p c t", c=len(hs)))
            k.dma(DST[:, 0, :, i * 128:(i + 1) * 128].rearrange("h p t -> p h t"), a[:])
            k.dma(DST[:, 1, 0:64, i * 128:(i + 1) * 128].rearrange("h p t -> p h t"), b[:])
    k.end()

    k.begin()
    KTa = k.sb([128, T], BF16)
    KTb = k.sb([64, T], BF16)
    QTa = k.sb([128, T], BF16)
    QTb = k.sb([64, T], BF16)
    Vt = k.sb([128, NT, 128], BF16)
    OTh = k.sb([128, T], BF16)
    ones = k.sb([128, 128], BF16)
    k.memset(ones[:], 1.0)
    PT = [k.sb([128, 512], BF16) for _ in range(4)]
    rec = [k.sb([128, 512], F32) for _ in range(2)]
    n = 0
    m = 0
    for h in range(12):
        k.dma(KTa[:], S["MKT"][h, 0, :, :])
        k.dma(KTb[:], S["MKT"][h, 1, 0:64, :])
        k.dma(QTa[:], S["MQT"][h, 0, :, :])
        k.dma(QTb[:], S["MQT"][h, 1, 0:64, :])
        k.dma(Vt[:], S["KV2"][:, h * 256 + 128:h * 256 + 256].rearrange("(t p) d -> p t d", p=128))
        if not need_ctx:
            k.memset(OTh[:, 0:CTX], 0.0)
        blocks = [(CTX + 512 * qb_, 512, NT) for qb_ in range(8)] + ([(0, 256, 2)] if need_ctx else [])
        for (q0, nq, nkt) in blocks:
            def score(kt, n_):
                Sp = k.ps[(0, 1, 6)[n_ % 3]]
                pt = PT[n_ % 4]
                k.matmul(Sp[:, 0:nq], KTa[:, kt * 128:(kt + 1) * 128], QTa[:, q0:q0 + nq], start=True, stop=False)
                k.matmul(Sp[:, 0:nq], KTb[:, kt * 128:(kt + 1) * 128], QTb[:, q0:q0 + nq], start=False, stop=True)
                k.act(pt[:, 0:nq], Sp[:, 0:nq], AF.Exp)

            po = k.ps[2 + 2 * (m % 2)]
            psm = k.ps[3 + 2 * (m % 2)]
            score(0, n)
            if nkt > 1:
                score(1, n + 1)
            for kt in range(nkt):
                if kt + 2 < nkt:
                    score(kt + 2, n + 2)
                pt = PT[n % 4]
                n += 1
                k.matmul(po[:, 0:nq], Vt[:, kt, :], pt[:, 0:nq], start=(kt == 0), stop=(kt == nkt - 1))
                k.matmul(psm[:, 0:nq], ones[:], pt[:, 0:nq], start=(kt == 0), stop=(kt == nkt - 1))
            rc_ = rec[m % 2]
            m += 1
            k.recip(rc_[:, 0:nq], psm[:, 0:nq])
            k.tt(OTh[:, q0:q0 + nq], po[:, 0:nq], rc_[:, 0:nq], ALU.mult)
        k.dma(OT[8 + h, :, :], OTh[:])
    k.end()


def gla_scratch(k):
    return dict(OF=k.dram("OF", [T, 1536], F32), OB=k.dram("OB", [T, 1536], F32))


def ph_gla(k, I, l, P, S, OT):
    k.begin()
    identf = k.sb([128, 128], F32)
    k.dma(identf[:], I["ident"][:])
    ib = load_ident_bf(k, I)
    Lm = [k.sb([64, 64], F32) for _ in range(2)]
    Um = [k.sb([64, 64], F32) for _ in range(2)]
    Mk = [k.sb([64, 64], F32) for _ in range(2)]
    w2 = [k.sb([17, 768], F32) for _ in range(2)]
    for d_ in range(2):
        k.dma(Lm[d_][:], I["gl_L"][d_, :, :])
        k.dma(Um[d_][:], I["gl_U"][d_, :, :])
        k.dma(Mk[d_][:], I["gl_M"][d_, :, :])
        nm = "f" if d_ == 0 else "b"
        k.dma(w2[d_][0:16, :], I["gla_w_gate_" + nm][l, :, :])
        k.dma(w2[d_][16:17, :], I["gla_b_gate_" + nm][l:l + 1, :])
    negs = k.sb([64, 1], F32)
    k.memset(negs[:], -1.0 / 16)
    NB = 3
    aT = [k.sb([17, 64], F32) for _ in range(NB)]
    for t_ in aT:
        k.memset(t_[:], 1.0)
    qin = [k.sb([64, 768], F32) for _ in range(NB)]
    kin = [k.sb([64, 768], F32) for _ in range(NB)]
    vin = [k.sb([64, 1536], BF16) for _ in range(NB)]
    ain = [k.sb([64, 16], F32) for _ in range(NB)]
    ez = [k.sb([64, 768], F32) for _ in range(2)]
    l6 = [k.sb([64, 768], F32) for _ in range(NB)]
    E1 = [k.sb([64, 768], F32) for _ in range(2)]
    E2 = [k.sb([64, 768], F32) for _ in range(2)]
    E3 = [k.sb([64, 768], F32) for _ in range(2)]
    eB = [k.sb([128, 6], F32) for _ in range(2)]
    qe = [k.sb([64, 768], BF16) for _ in range(2)]
    ke = [k.sb([64, 768], BF16) for _ in range(2)]
    kend = [k.sb([64, 768], BF16) for _ in range(2)]
    qkT = [k.sb([128, 128], BF16) for _ in range(6)]
    attT = [k.sb([64, 64], BF16) for _ in range(6)]
    osb = [k.sb([64, 1536], F32) for _ in range(NB)]
    Sf = [[k.sb([128, 256], F32) for _ in range(6)] for _ in range(2)]
    Sb = [[k.sb([128, 256], BF16) for _ in range(6)] for _ in range(2)]
    for d_ in range(2):
        for h in range(6):
            k.memset(Sf[d_][h][:], 0.0)
            k.memset(Sb[d_][h][:], 0.0)
    orders = [list(range(68)), [3, 2, 1, 0] + list(range(67, 3, -1))]
    nn = 0
    cc = 0
    for ci in range(68):
        for d_ in range(2):
            DST = S["OF"] if d_ == 0 else S["OB"]
            aoff = O_AF if d_ == 0 else O_AB
            c = orders[d_][ci]
            t0 = c * 64
            sl = cc % NB
            s2 = cc % 2
            cc += 1
            q, kk, v, a, at, lg, o = qin[sl], kin[sl], vin[sl], ain[sl], aT[sl], l6[sl], osb[sl]
            ezz = ez[s2]
            e1, e2, e3, eb = E1[s2], E2[s2], E3[s2], eB[s2]
            qe_, ke_, kd_ = qe[s2], ke[s2], kend[s2]
            k.dma(q[:], P[t0:t0 + 64, O_GQ:O_GQ + 768])
            k.dma(kk[:], P[t0:t0 + 64, O_GK:O_GK + 768])
            k.dma(v[:], P[t0:t0 + 64, O_GV:O_GV + 1536])
            k.dma(a[:], P[t0:t0 + 64, aoff:aoff + 16])
            pz = (k.ps[0], k.ps[1])
            pB = (k.ps[2], k.ps[3])
            k.transpose(pz[0][0:16, 0:64], a[:], identf[0:64, 0:64])
            k.copy(at[0:16, :], pz[0][0:16, 0:64], eng="dve")
            for hh in range(2):
                k.matmul(pz[hh][0:64, 0:384], at[:], w2[d_][:, hh * 384:(hh + 1) * 384])
            for hh in range(2):
                k.act(ezz[:, hh * 384:(hh + 1) * 384], pz[hh][0:64, 0:384], AF.Exp, scale=-1.0)
            k.act(lg[:], ezz[:], AF.Ln, bias=1.0)
            for hh in range(2):
                k.matmul(pB[hh][0:64, 0:384], Lm[d_][:], lg[:, hh * 384:(hh + 1) * 384])
            for hh in range(2):
                k.matmul(pz[hh][0:64, 0:384], Um[d_][:], lg[:, hh * 384:(hh + 1) * 384])
            for h in range(6):
                k.matmul(pB[1][:, 384 + h:385 + h], lg[:, h * 128:(h + 1) * 128], negs[:])
            for hh in range(2):
                k.act(e1[:, hh * 384:(hh + 1) * 384], pB[hh][0:64, 0:384], AF.Exp)
                k.act(e2[:, hh * 384:(hh + 1) * 384], pB[hh][0:64, 0:384], AF.Exp, scale=-1.0)
                k.act(e3[:, hh * 384:(hh + 1) * 384], pz[hh][0:64, 0:384], AF.Exp)
            k.act(eb[:], pB[1][:, 384:390], AF.Exp)
            k.stt(qe_[:], q[:], 128.0 ** -0.5, e1[:], ALU.mult, ALU.mult)
            k.tt(ke_[:], kk[:], e2[:], ALU.mult)
            k.tt(kd_[:], kk[:], e3[:], ALU.mult)
            b4, b5, b6, b7 = k.ps[4], k.ps[5], k.ps[6], k.ps[7]
            outv = (b5[0:64, 0:256], b5[0:64, 256:512], b6[0:64, 0:256])
            uv = (b6[:, 256:512], b7[:, 0:256], b7[:, 256:512])
            for g in range(2):
                hs_ = [3 * g + j for j in range(3)]
                ptp = b4[:, 0:192].bitcast(BF16)
                qks = [qkT[(nn + j) % 6] for j in range(3)]
                ats = [attT[(nn + j) % 6] for j in range(3)]
                nn += 3
                for j, h in enumerate(hs_):
                    hs = slice(h * 128, (h + 1) * 128)
                    k.transpose(ptp[:, j * 128:j * 128 + 64], qe_[:, hs], ib[0:64, 0:64], inc=False)
                    k.transpose(ptp[:, j * 128 + 64:(j + 1) * 128], ke_[:, hs], ib[0:64, 0:64], inc=(j == 2))
                for j, h in enumerate(hs_):
                    k.copy(qks[j][:], ptp[:, j * 128:(j + 1) * 128], eng="dve")
                for j, h in enumerate(hs_):
                    k.matmul(b4[0:64, 256 + j * 64:256 + (j + 1) * 64], qks[j][:, 64:128], qks[j][:, 0:64])
                for j, h in enumerate(hs_):
                    k.tt(ats[j][:], b4[0:64, 256 + j * 64:256 + (j + 1) * 64], Mk[d_][:], ALU.mult)
                for j, h in enumerate(hs_):
                    k.matmul(outv[j], qks[j][:, 0:64], Sb[d_][h][:], start=True, stop=False)
                    k.matmul(outv[j], ats[j][:], v[:, h * 256:(h + 1) * 256], start=False, stop=True)
                for j, h in enumerate(hs_):
                    k.copy(o[:, h * 256:(h + 1) * 256], outv[j], eng="act")
                    k.matmul(uv[j], kd_[:, h * 128:(h + 1) * 128], v[:, h * 256:(h + 1) * 256])
                for j, h in enumerate(hs_):
                    k.stt(Sf[d_][h][:], Sf[d_][h][:], eb[:, h:h + 1], uv[j], ALU.mult, ALU.add)
                    k.copy(Sb[d_][h][:], Sf[d_][h][:], eng="dve")
            k.dma(DST[t0:t0 + 64, :], o[:])
    k.end()

    k.begin()
    ib = load_ident_bf(k, I)
    G = k.sb([128, 6, 256], F32)
    for h in range(6):
        k.dma(G[:, h, :], I["gla_out_norm"][l, :].pbc(128))
    of = [k.sb([128, 1536], F32) for _ in range(2)]
    ob = [k.sb([128, 1536], F32) for _ in range(2)]
    gg = [k.sb([128, 1536], F32) for _ in range(2)]
    tmp = k.sb([128, 1536], F32)
    on = k.sb([128, 1536], F32)
    yb = [k.sb([128, 1536], BF16) for _ in range(2)]
    ss = k.sb([128, 6], F32)
    oT = [k.sb([128, 12, 128], BF16) for _ in range(2)]
    for n, i in enumerate(range(NT)):
        a, b, g, y, o = of[n % 2], ob[n % 2], gg[n % 2], yb[n % 2], oT[n % 2]
        k.dma(a[:], S["OF"][i * 128:(i + 1) * 128, :])
        k.dma(b[:], S["OB"][i * 128:(i + 1) * 128, :])
        k.dma(g[:], P[i * 128:(i + 1) * 128, O_GG:O_GG + 1536])
        k.tt(a[:], a[:], b[:], ALU.add)
        head_rms(k, on[:].rearrange("p (h d) -> p h d", h=6), a[:].rearrange("p (h d) -> p h d", h=6), G[:], ss[:],
                 tmp[:].rearrange("p (h d) -> p h d", h=6), 6, 256)
        k.act(g[:], g[:], AF.Silu)
        k.tt(y[:], on[:], g[:], ALU.mult)
        for gi in range(2):
            ps = k.ps[(2 * n + gi) % 4][:].bitcast(BF16)
            cn = 8 if gi == 0 else 4
            for c in range(cn):
                cc = gi * 8 + c
                k.transpose(ps[:, c * 128:(c + 1) * 128], y[:, cc * 128:(cc + 1) * 128], ib[:], inc=(c == cn - 1))
            k.copy(o[:, gi * 8:gi * 8 + cn, :], ps[:, 0:cn * 128].rearrange("p (c t) -> p c t", c=cn))
        k.dma(OT[20:32, :, i * 128:(i + 1) * 128].rearrange("c p t -> p c t"), o[:])
    k.end()


def resid_gemm(k, AT, Kc, W, MOD, l, gi, Xin, Xout, tiles, slab=4):
    rs_needed = sorted({1 if i < 2 else 0 for i in tiles})

    def setup():
        gb = {}
        for r in rs_needed:
            gb[r] = k.sb([128, D], F32)
            k.dma(gb[r][:], MOD[l, r, gi * D:(gi + 1) * D].pbc(128))
        return dict(gb=gb, xo=[k.sb([128, 512], F32) for _ in range(3)], xn=[k.sb([128, 512], F32) for _ in range(3)],
                    n=0)

    def ep(c, i, n0, nn, ps):
        r = 1 if i < 2 else 0
        xo = c["xo"][c["n"] % 3]
        xn = c["xn"][c["n"] % 3]
        c["n"] += 1
        k.dma(xo[:, 0:nn], Xin[i * 128:(i + 1) * 128, n0:n0 + nn])
        k.tt(xn[:, 0:nn], ps, c["gb"][r][:, n0:n0 + nn], ALU.mult)
        k.tt(xn[:, 0:nn], xn[:, 0:nn], xo[:, 0:nn], ALU.add)
        k.dma(Xout[i * 128:(i + 1) * 128, n0:n0 + nn], xn[:, 0:nn])

    gemm(k, AT, Kc, W, D, tiles, ep, slab=slab, setup=setup)


def ph_moe_up(k, I, l, HT, GATET, GP, tiles, slab=9):
    k.begin()
    at = k.sb([128, 32, slab * 128], BF16)
    gts = k.sb([16, slab * 128], F32)
    selT = k.sb([16, 16, 128], F32)
    k.dma(selT[:], I["selT"][:].rearrange("e k m -> k e m"))
    GB = k.sb([128, slab * 128], F32)
    w1g = [k.sb([128, 32, 256], BF16) for _ in range(2)]
    w3g = [k.sb([128, 32, 256], BF16) for _ in range(2)]
    sa = [k.sb([128, 512], F32) for _ in range(2)]
    tm = [k.sb([128, 512], F32) for _ in range(2)]
    go = [k.sb([128, 512], BF16) for _ in range(3)]
    nw = 0
    nx = 0
    for s0 in range(0, len(tiles), slab):
        st = tiles[s0:s0 + slab]
        tok0 = st[0] * 128
        ntok = len(st) * 128
        groups = [(o_, min(512, ntok - o_)) for o_ in range(0, ntok, 512)]
        k.dma(at[:, :, 0:ntok], HT[:, :, tok0:tok0 + ntok].rearrange("c p t -> p c t"))
        k.dma(gts[:, 0:ntok], GATET[:, tok0:tok0 + ntok])
        for e in range(16):
            for (o_, n_) in groups:
                k.matmul(k.ps[0][:, 0:n_], selT[:, e, :], gts[:, o_:o_ + n_])
                k.copy(GB[:, o_:o_ + n_], k.ps[0][:, 0:n_], eng="dve")
            for fg in range(4):
                w1 = w1g[nw % 2]
                w3 = w3g[nw % 2]
                nw += 1
                for (w, nm) in ((w1, "moe_w1"), (w3, "moe_w3")):
                    for c0 in (0, 16):
                        k.dma(w[:, c0:c0 + 16, :],
                              I[nm][l, e, c0 * 128:(c0 + 16) * 128, fg * 256:(fg + 1) * 256].rearrange("(c p) n -> p c n", p=128))
                for f2 in range(2):
                    for (o_, n_) in groups:
                        pa = k.ps[2 + nx % 2]
                        pb = k.ps[4 + nx % 2]
                        s_ = sa[nx % 2]
                        t_ = tm[nx % 2]
                        g_ = go[nx % 3]
                        nx += 1
                        for c in range(32):
                            k.matmul(pa[:, 0:n_], w1[:, c, f2 * 128:(f2 + 1) * 128], at[:, c, o_:o_ + n_],
                                     start=(c == 0), stop=(c == 31))
                        for c in range(32):
                            k.matmul(pb[:, 0:n_], w3[:, c, f2 * 128:(f2 + 1) * 128], at[:, c, o_:o_ + n_],
                                     start=(c == 0), stop=(c == 31))
                        k.act(s_[:, 0:n_], pa[:, 0:n_], AF.Silu)
                        k.tt(t_[:, 0:n_], s_[:, 0:n_], pb[:, 0:n_], ALU.mult)
                        k.tt(g_[:, 0:n_], t_[:, 0:n_], GB[:, o_:o_ + n_], ALU.mult)
                        k.dma(GP[e * 8 + fg * 2 + f2, :, tok0 + o_:tok0 + o_ + n_], g_[:, 0:n_])
    k.end()


def ph_moe_down(k, I, l, GP, MOD, Xa, Xb, tiles):
    W2 = I["moe_w2"][l].rearrange("e f n -> (e f) n")
    src, dst = Xa, Xb
    for q in range(4):
        resid_gemm(k, GP[q * 32:(q + 1) * 32], 32, W2[q * 4096:(q + 1) * 4096, :], MOD, l, 5, src, dst, tiles)
        src, dst = dst, src
    return src


INPUT_SHAPES = None


def build_program(shapes):
    k = K()
    I = {}
    for nm, shp in shapes.items():
        I[nm] = k.dram(nm, list(shp), F32, kind="ExternalInput")
    OUT = k.dram("out", [SEQ, D], F32, kind="ExternalOutput")
    XA = k.dram("XA", [T, D], F32)
    XB = k.dram("XB", [T, D], F32)
    MOD = k.dram("MOD", [DEPTH, 2, 6 * D], F32)
    HT = k.dram("HT", [32, 128, T], BF16)
    P = k.dram("P", [T, IN_W], F32)
    OT = k.dram("OT", [32, 128, T], BF16)
    NQT = k.dram("NQT", [8, 128, T], BF16)
    NKT = k.dram("NKT", [8, 128, T], BF16)
    GATE = k.dram("GATE", [T, 16], F32)
    GATET = k.dram("GATET", [16, T], F32)
    GP = k.dram("GP", [128, 128, T], BF16)
    MS = mla_scratch(k)
    GS = gla_scratch(k)
    k.dma(XA[0:CTX, :], I["ctx"][:], q="sp")
    for j in range(4):
        k.dma(XA[CTX + j * 1024:CTX + (j + 1) * 1024, :], I["x"][j * 1024:(j + 1) * 1024, :], q="sp")
    ph_ada(k, I, MOD)
    Xc, Xo = XA, XB
    allt = list(range(NT))
    latt = list(range(2, NT))
    for l in range(DEPTH):
        need_ctx = l < DEPTH - 1
        tl = allt if need_ctx else latt
        ph_norm(k, I, l, 0, Xc, MOD, HT, allt)
        ph_proj_in(k, I, l, HT, P, allt)
        ph_na_prep(k, I, l, P, NQT, NKT, allt)
        ph_na_attn(k, I, l, P, NQT, NKT, OT, need_ctx)
        ph_mla(k, I, l, P, MS, OT, need_ctx)
        ph_gla(k, I, l, P, GS, OT)
        resid_gemm(k, OT, 32, I["w_out"][l], MOD, l, 2, Xc, Xo, tl)
        ph_norm(k, I, l, 1, Xo, MOD, HT, tl, router=dict(GATE=GATE, GATET=GATET))
        ph_moe_up(k, I, l, HT, GATET, GP, tl, slab=(9 if need_ctx else 8))
        fin = ph_moe_down(k, I, l, GP, MOD, Xo, Xc, tl)
        Xc, Xo = (fin, Xc if fin is Xo else Xo)
    for j in range(4):
        k.dma(OUT[j * 1024:(j + 1) * 1024, :], Xc[CTX + j * 1024:CTX + (j + 1) * 1024, :], q="sp")
    k.barrier()
    return k


def kernel(**inputs):
    inp = {n: np.asarray(v) for n, v in inputs.items()}
    B = inp["x"].shape[0]
    consts = host_consts(inp)
    shared = {}
    for nm in ("w_ada", "b_ada", "norm1", "norm2", "w_in", "w_out", "na_q_norm", "na_k_norm", "mla_qa_norm",
               "mla_kva_norm", "mla_w_uq", "mla_w_ukv", "mla_q_norm", "mla_k_norm", "gla_w_gate_f", "gla_b_gate_f",
               "gla_w_gate_b", "gla_b_gate_b", "gla_out_norm", "router_bias", "moe_w1", "moe_w3", "moe_w2"):
        shared[nm] = np.ascontiguousarray(inp[nm], dtype=np.float32)
    shared["wrT"] = np.ascontiguousarray(inp["w_router"].reshape(32, 128, 16).transpose(1, 0, 2), dtype=np.float32)
    for nm, v in consts.items():
        shared[nm] = np.ascontiguousarray(v, dtype=np.float32)
    in_maps = []
    for b in range(B):
        m = dict(shared)
        m["x"] = np.ascontiguousarray(inp["x"][b], dtype=np.float32)
        m["ctx"] = np.ascontiguousarray(inp["ctx"][b], dtype=np.float32)
        cvec = np.stack([inp["c"][b], inp["c_ctx"]], 0).astype(np.float32)
        m["cT"] = np.ascontiguousarray(cvec.reshape(2, 32, 128).transpose(2, 1, 0))
        in_maps.append(m)
    shapes = {nm: v.shape for nm, v in in_maps[0].items()}
    k = build_program(shapes)
    res = run_bass_kernel_spmd(k.nc, in_maps, core_ids=list(range(B)))
    return np.stack([res.results[b]["out"] for b in range(B)], 0).astype(np.float32)
```

```python
import numpy as np
from contextlib import ExitStack
import concourse.bass as bass
import concourse.mybir as mybir
from concourse.bass_utils import run_bass_kernel_spmd

F32 = mybir.dt.float32
BF16 = mybir.dt.bfloat16
AF = mybir.ActivationFunctionType
ALU = mybir.AluOpType
AX = mybir.AxisListType

COMPUTE = ("pe", "act", "dve", "pool")

D = 4096
SEQ = 4096
CTX = 256
T = SEQ + CTX
NT = T // 128
DEPTH = 2
EPS = 1e-6
IN_W = 9312
O_NAQ, O_NAK, O_NAV = 0, 1024, 2048
O_CQ, O_CKV, O_KR = 3072, 4096, 4608
O_GQ, O_GK, O_GV, O_GG, O_AF, O_AB = 4672, 5440, 6208, 7744, 9280, 9296
NEG = -30000.0
import os
RDBG = int(os.environ.get('RDBG', '0'))
RSKIP = os.environ.get('RSKIP', '')


class TT:
    def __init__(self, k, h, name):
        self.k = k
        self.h = h
        self.name = name
        self.w = None
        self.r = []
        self.dslot = None
        self.psum = False

    def __getitem__(self, idx):
        return V(self, self.h[idx])


class V:
    def __init__(self, t, ap):
        self.t = t
        self.ap = ap

    def __getitem__(self, idx):
        return V(self.t, self.ap[idx])

    def rearrange(self, *a, **kw):
        return V(self.t, self.ap.rearrange(*a, **kw))

    def bitcast(self, dt):
        return V(self.t, self.ap.bitcast(dt))

    def pbc(self, n):
        return V(self.t, self.ap.partition_broadcast(n))

    def bc(self, shape):
        return V(self.t, self.ap.to_broadcast(list(shape)))


class K:
    def __init__(self):
        self.nc = bass.Bass("TRN2", target_bir_lowering=False)
        nc = self.nc
        self.E = dict(pe=nc.tensor, act=nc.scalar, dve=nc.vector, pool=nc.gpsimd, sp=nc.sync)
        self.sems = {}
        self.cnt = {}
        for e in COMPUTE:
            self.sems[e] = nc.alloc_semaphore("c_" + e)
            self.cnt[e] = 0
        self.waited = {e: {} for e in self.E}
        self.nid = 0
        self.ninst = 0
        self.dpool = []
        self.stack = None
        self.phase_tiles = []
        self.ps = [TT(self, nc.alloc_psum_tensor(f"psb{i}", [128, 512], F32), f"psb{i}") for i in range(8)]
        self.rr = 0
        for t in self.ps:
            t.psum = True

    def begin(self):
        self.stack = ExitStack()
        self.phase_tiles = []

    def end(self):
        self.barrier()
        for t in self.phase_tiles:
            if t.dslot is not None:
                self.dpool.append(t.dslot)
                t.dslot = None
        self.stack.close()
        self.stack = None

    def sb(self, shape, dt=F32, name=None):
        self.nid += 1
        name = (name or "sb") + str(self.nid)
        h = self.stack.enter_context(self.nc.sbuf_tensor(name, list(shape), dt))
        t = TT(self, h, name)
        self.phase_tiles.append(t)
        return t

    def dram(self, name, shape, dt=F32, kind="Internal"):
        h = self.nc.dram_tensor(name, list(shape), dt, kind=kind)
        return TT(self, h, name)

    def _slot(self, t):
        if t.dslot is None:
            if self.dpool:
                t.dslot = self.dpool.pop()
            else:
                self.nid += 1
                key = "d%d" % self.nid
                self.sems[key] = self.nc.alloc_semaphore(key)
                self.cnt[key] = 0
                t.dslot = key
        return t.dslot

    def _wait(self, eng, dep):
        if dep is None:
            return
        key, val = dep
        if eng == "pe" and key == "pe":
            return
        if self.waited[eng].get(key, 0) >= val:
            return
        self.waited[eng][key] = val
        self.E[eng].wait_ge(self.sems[key], val)

    def _deps(self, eng, reads, writes):
        for v in reads:
            if v is not None:
                self._wait(eng, v.t.w)
                if v.t.psum:
                    for d in v.t.r:
                        if d[0] != eng:
                            self._wait(eng, d)
        for v in writes:
            t = v.t
            self._wait(eng, t.w)
            for d in t.r:
                self._wait(eng, d)

    def _done(self, dep, reads, writes):
        for v in reads:
            if v is None:
                continue
            r = v.t.r
            r.append(dep)
            if len(r) > 16:
                best = {}
                for kk, vv in r:
                    best[kk] = max(best.get(kk, 0), vv)
                v.t.r = list(best.items())
        for v in writes:
            v.t.w = dep
            v.t.r = []

    def op(self, eng, fn, reads, writes, inc=True):
        self._deps(eng, reads, writes)
        ins = fn()
        self.ninst += 1
        if inc:
            self.cnt[eng] += 1
            ins.then_inc(self.sems[eng], 1)
            self._done((eng, self.cnt[eng]), reads, writes)
        else:
            self._done((eng, self.cnt[eng] + 1), reads, writes)
        return ins

    def dma(self, out, in_, q=None, **kw):
        if q is None:
            sb_out = out.t in self.phase_tiles or out.t in self.ps
            q = "sp" if sb_out else "act"
            if out.ap.dtype != in_.ap.dtype:
                q = "pool"
        owner = out.t
        key = self._slot(owner)
        self._deps(q, [in_], [out])
        ins = self.E[q].dma_start(out=out.ap, in_=in_.ap, **kw)
        self.cnt[key] += 16
        ins.then_inc(self.sems[key], 16)
        self._done((key, self.cnt[key]), [in_], [out])
        self.ninst += 1
        return ins

    def barrier(self):
        for eng in self.E:
            for key, c in self.cnt.items():
                if c > 0:
                    self._wait(eng, (key, c))

    def matmul(self, out, lhsT, rhs, start=True, stop=True):
        return self.op("pe", lambda: self.nc.tensor.matmul(out.ap, lhsT.ap, rhs.ap, start=start, stop=stop),
                       [lhsT, rhs], [out], inc=stop)

    def transpose(self, out, in_, ident, inc=True):
        return self.op("pe", lambda: self.nc.tensor.transpose(out.ap, in_.ap, ident.ap), [in_, ident], [out], inc=inc)

    def act(self, out, in_, func, bias=None, scale=None, accum=None):
        kw = {}
        reads = [in_]
        writes = [out]
        if bias is not None:
            if isinstance(bias, V):
                kw["bias"] = bias.ap
                reads.append(bias)
            else:
                kw["bias"] = bias
        if scale is not None:
            if isinstance(scale, V):
                kw["scale"] = scale.ap
                reads.append(scale)
            else:
                kw["scale"] = scale
        if accum is not None:
            kw["accum_out"] = accum.ap
            writes.append(accum)
        return self.op("act", lambda: self.nc.scalar.activation(out.ap, in_.ap, func, **kw), reads, writes)

    def copy(self, out, in_, eng=None):
        if eng is None:
            self.rr ^= 1
            eng = "dve" if self.rr else "act"
        if eng == "act":
            return self.act(out, in_, AF.Copy)
        return self.op(eng, lambda: self.E[eng].tensor_copy(out.ap, in_.ap), [in_], [out])

    def tt(self, out, a, b, op, eng="dve"):
        return self.op(eng, lambda: self.E[eng].tensor_tensor(out.ap, a.ap, b.ap, op), [a, b], [out])

    def ts(self, out, a, s1, op0, s2=None, op1=None, accum=None, eng="dve"):
        reads = [a]
        writes = [out]
        s1a = s1.ap if isinstance(s1, V) else s1
        s2a = s2.ap if isinstance(s2, V) else s2
        if isinstance(s1, V):
            reads.append(s1)
        if isinstance(s2, V):
            reads.append(s2)
        kw = {}
        if op1 is not None:
            kw["op1"] = op1
        if accum is not None:
            kw["accum_out"] = accum.ap
            writes.append(accum)
        return self.op(eng, lambda: self.E[eng].tensor_scalar(out.ap, a.ap, s1a, s2a, op0, **kw), reads, writes)

    def stt(self, out, a, s, b, op0, op1, eng="dve"):
        reads = [a, b]
        sa = s.ap if isinstance(s, V) else s
        if isinstance(s, V):
            reads.append(s)
        return self.op(eng, lambda: self.E[eng].scalar_tensor_tensor(out.ap, a.ap, sa, b.ap, op0, op1),
                       reads, [out])

    def reduce(self, out, in_, op, axis=AX.X, eng="dve"):
        return self.op(eng, lambda: self.E[eng].tensor_reduce(out.ap, in_.ap, axis, op), [in_], [out])

    def recip(self, out, in_, eng="dve"):
        return self.op(eng, lambda: self.E[eng].reciprocal(out.ap, in_.ap), [in_], [out])

    def memset(self, out, val, eng="dve"):
        return self.op(eng, lambda: self.E[eng].memset(out.ap, val), [], [out])

    def rstd(self, out, ss, n):
        self.ts(out, ss, 1.0 / n, ALU.mult, EPS, ALU.add)
        self.act(out, out, AF.Sqrt)
        self.recip(out, out)


def ph_ada(k, I, MOD):
    k.begin()
    cT = k.sb([128, 32, 2], F32)
    k.dma(cT[:], I["cT"][:])
    sT = k.sb([128, 32, 2], BF16)
    k.act(sT[:], cT[:], AF.Silu)
    wt = [k.sb([128, 8, 512], BF16) for _ in range(4)]
    bt = [k.sb([2, 512], F32) for _ in range(2)]
    ot = [k.sb([2, 512], F32) for _ in range(2)]
    n = 0
    for l in range(DEPTH):
        for nt in range(48):
            ps = k.ps[nt % 2]
            for kq in range(4):
                w = wt[n % 4]
                n += 1
                src = I["w_ada"][l, kq * 1024:(kq + 1) * 1024, nt * 512:(nt + 1) * 512]
                k.dma(w[:], src.rearrange("(c p) n -> p c n", p=128))
                for c in range(8):
                    kc = kq * 8 + c
                    k.matmul(ps[0:2, :], sT[:, kc, :], w[:, c, :], start=(kc == 0), stop=(kc == 31))
            b = bt[nt % 2]
            o = ot[nt % 2]
            k.dma(b[:], I["b_ada"][l:l + 1, nt * 512:(nt + 1) * 512].pbc(2) if False else
                  I["b_ada"][l, nt * 512:(nt + 1) * 512].pbc(2))
            k.tt(o[:], ps[0:2, :], b[:], ALU.add)
            k.dma(MOD[l, :, nt * 512:(nt + 1) * 512], o[:])
    k.end()


def ph_norm(k, I, l, which, X, MOD, HT, tiles, router=None):
    k.begin()
    gain = I["norm1" if which == 0 else "norm2"]
    sh_i, sc_i = (0, 1) if which == 0 else (3, 4)
    ident = k.sb([128, 128], F32)
    k.dma(ident[:], I["ident"][:])
    tmps = [k.sb([128, D], F32) for _ in range(2)]
    hfs = [k.sb([128, D], F32) for _ in range(2)]
    tmp, hf = tmps[0], hfs[0]
    A = [k.sb([128, D], F32) for _ in range(2)]
    Bv = [k.sb([128, D], F32) for _ in range(2)]
    k.dma(hf[:], gain[l, :].pbc(128))
    for r in range(2):
        k.dma(tmp[:], MOD[l, r, sc_i * D:(sc_i + 1) * D].pbc(128))
        k.dma(Bv[r][:], MOD[l, r, sh_i * D:(sh_i + 1) * D].pbc(128))
        k.stt(A[r][:], tmp[:], 1.0, hf[:], ALU.add, ALU.mult)
    xb = [k.sb([128, D], F32) for _ in range(2)]
    hT = [k.sb([128, 32, 128], BF16) for _ in range(2)]
    ss = [k.sb([128, 1], F32) for _ in range(2)]
    if router is not None:
        wr = k.sb([128, 32, 16], F32)
        k.dma(wr[:], I["wrT"][:])
        rb = k.sb([128, 16], F32)
        k.dma(rb[:], I["router_bias"][:].pbc(128))
        hTf = [k.sb([128, 4, 128], F32) for _ in range(2)]
        sc = k.sb([128, 16], F32)
        lg = k.sb([128, 16], F32)
        bs = k.sb([128, 16], F32)
        m1 = k.sb([128, 4], F32)
        m2 = k.sb([128, 4], F32)
        t16 = k.sb([128, 16], F32)
        gs = k.sb([128, 4], F32)
        gm = k.sb([128, 1], F32)
        sel = k.sb([128, 16], F32)
        gt = [k.sb([128, 16], F32) for _ in range(2)]
        gTt = [k.sb([16, 128], F32) for _ in range(2)]
    for n, i in enumerate(tiles):
        r = 1 if i < 2 else 0
        x = xb[n % 2]
        s = ss[n % 2]
        h = hT[n % 2]
        tmp, hf = tmps[n % 2], hfs[n % 2]
        k.dma(x[:], X[i * 128:(i + 1) * 128, :])
        k.memset(s[:], 0.0)
        k.act(tmp[:], x[:], AF.Square, accum=s[:])
        k.rstd(s[:], s[:], D)
        k.stt(tmp[:], x[:], s[:, 0:1], A[r][:], ALU.mult, ALU.mult)
        k.tt(hf[:], tmp[:], Bv[r][:], ALU.add)
        psr = k.ps[7]
        for c4 in range(8):
            ps = k.ps[c4 % 4]
            for j in range(4):
                c = c4 * 4 + j
                k.transpose(ps[:, j * 128:(j + 1) * 128], hf[:, c * 128:(c + 1) * 128], ident[:], inc=(j == 3))
            ceng = "dve" if c4 % 2 else "act"
            k.copy(h[:, c4 * 4:(c4 + 1) * 4, :], ps[:].rearrange("p (j t) -> p j t", j=4), eng=ceng)
            if router is not None:
                hx = hTf[c4 % 2]
                if 'h' not in RSKIP:
                    k.copy(hx[:], ps[:].rearrange("p (j t) -> p j t", j=4), eng=ceng)
                for j in range(4):
                    c = c4 * 4 + j
                    if RDBG == 3:
                        continue
                    k.matmul(psr[:, c * 16:(c + 1) * 16], hx[:, j, :], wr[:, c, :], start=True, stop=True)
        k.dma(HT[:, :, i * 128:(i + 1) * 128].rearrange("c p t -> p c t"), h[:])
        if router is not None:
            g = gt[n % 2]
            if 'r' not in RSKIP:
                k.reduce(lg[:], psr[:, :].rearrange("p (c e) -> p e c", e=16), ALU.add)
            else:
                k.memset(lg[:], 0.5)
            if 's' not in RSKIP:
                k.act(sc[:], lg[:], AF.Sigmoid)
            else:
                k.copy(sc[:], lg[:], eng='dve')
            if RDBG in (1, 3):
                k.dma(router["GATE"][i * 128:(i + 1) * 128, :], sc[:])
                continue
            k.tt(bs[:], sc[:], rb[:], ALU.add)
            bs3 = bs[:].rearrange("p (g e) -> p g e", g=4)
            k.reduce(m1[:], bs3, ALU.max)
            k.tt(t16[:].rearrange("p (g e) -> p g e", g=4), bs3, m1[:].bc([128, 4, 4]) if False else
                 m1[:].rearrange("p (g o) -> p g o", o=1).bc([128, 4, 4]), ALU.is_equal)
            k.stt(t16[:], t16[:], -1e4, bs[:], ALU.mult, ALU.add)
            k.reduce(m2[:], t16[:].rearrange("p (g e) -> p g e", g=4), ALU.max)
            k.tt(gs[:], m1[:], m2[:], ALU.add)
            k.reduce(gm[:], gs[:], ALU.max)
            k.ts(gs[:], gs[:], gm[:, 0:1], ALU.is_equal)
            k.tt(sel[:].rearrange("p (g e) -> p g e", g=4), bs3,
                 m2[:].rearrange("p (g o) -> p g o", o=1).bc([128, 4, 4]), ALU.is_ge)
            k.tt(sel[:].rearrange("p (g e) -> p g e", g=4), sel[:].rearrange("p (g e) -> p g e", g=4),
                 gs[:].rearrange("p (g o) -> p g o", o=1).bc([128, 4, 4]), ALU.mult)
            k.tt(sel[:], sel[:], sc[:], ALU.mult)
            k.reduce(gm[:], sel[:], ALU.add)
            k.recip(gm[:], gm[:])
            k.ts(g[:], sel[:], gm[:, 0:1], ALU.mult)
            k.dma(router["GATE"][i * 128:(i + 1) * 128, :], g[:])
            if RDBG == 2:
                continue
            k.transpose(k.ps[6][0:16, 0:128], g[:], ident[:])
            gT = gTt[n % 2]
            k.copy(gT[:], k.ps[6][0:16, 0:128], eng="dve")
            k.dma(router["GATET"][:, i * 128:(i + 1) * 128], gT[:])
    k.end()


def gemm(k, AT, Kc, W, N, tiles, epilogue, slab=4, wdt=BF16, ng=512, setup=None, nat=2):
    k.begin()
    ectx = setup() if setup else None
    slabs = [tiles[s0:s0 + slab] for s0 in range(0, len(tiles), slab)]
    maxs = max(len(st) for st in slabs)
    at = [k.sb([128, Kc, maxs * 128], BF16) for _ in range(nat)]
    wt = [k.sb([128, Kc, ng], wdt) for _ in range(2)]
    nw = 0
    na = 0
    npb = 0
    for n0 in range(0, N, ng):
        nn = min(ng, N - n0)
        w = wt[nw % 2]
        nw += 1
        hc = max(1, Kc // 2)
        for c0 in range(0, Kc, hc):
            c1 = min(Kc, c0 + hc)
            k.dma(w[:, c0:c1, 0:nn], W[c0 * 128:c1 * 128, n0:n0 + nn].rearrange("(c p) n -> p c n", p=128))
        for st in slabs:
            assert st == list(range(st[0], st[0] + len(st)))
            ntok = len(st) * 128
            a = at[na % nat]
            na += 1
            k.dma(a[:, :, 0:ntok], AT[:, :, st[0] * 128:st[0] * 128 + ntok].rearrange("c p t -> p c t"))
            for j, i in enumerate(st):
                ps = k.ps[npb % 6]
                npb += 1
                for c in range(Kc):
                    k.matmul(ps[:, 0:nn], a[:, c, j * 128:(j + 1) * 128], w[:, c, 0:nn],
                             start=(c == 0), stop=(c == Kc - 1))
                epilogue(ectx, i, n0, nn, ps[:, 0:nn])
    k.end()


def ph_proj_in(k, I, l, HT, P, tiles):
    def setup():
        return [k.sb([128, 512], F32) for _ in range(3)]

    def ep(ob, i, n0, nn, ps):
        o = ob[(i + n0 // 512) % 3]
        k.copy(o[:, 0:nn], ps)
        k.dma(P[i * 128:(i + 1) * 128, n0:n0 + nn], o[:, 0:nn])

    gemm(k, HT, 32, I["w_in"][l], IN_W, tiles, ep, setup=setup, nat=3)


def load_ident_bf(k, I):
    ib = k.sb([128, 128], BF16)
    k.dma(ib[:], I["ident"][:])
    return ib


def head_rms(k, dst, src3, gain3, ss, tmp3, nh, dh, extra=None):
    k.tt(tmp3, src3, src3, ALU.mult)
    k.reduce(ss, tmp3, ALU.add)
    n = dh
    if extra is not None:
        k.ts(ss, ss, extra[0], ALU.add)
        n = extra[1]
    k.rstd(ss, ss, n)
    k.tt(tmp3, src3, ss.rearrange("p (h o) -> p h o", o=1).bc([128, nh, dh]), ALU.mult)
    k.tt(dst, tmp3, gain3, ALU.mult)


def ph_na_prep(k, I, l, P, NQT, NKT, tiles):
    k.begin()
    ib = load_ident_bf(k, I)
    Gq = k.sb([128, 8, 128], F32)
    Gk = k.sb([128, 8, 128], F32)
    for h in range(8):
        k.dma(Gq[:, h, :], I["na_q_norm"][l, :].pbc(128))
        k.dma(Gk[:, h, :], I["na_k_norm"][l, :].pbc(128))
    k.ts(Gq[:], Gq[:], 128.0 ** -0.5, ALU.mult)
    xin = [k.sb([128, 1024], F32) for _ in range(2)]
    tmp = k.sb([128, 1024], F32)
    xn = [k.sb([128, 1024], BF16) for _ in range(2)]
    ss = k.sb([128, 8], F32)
    xT = [k.sb([128, 8, 128], BF16) for _ in range(2)]
    n = 0
    for i in tiles:
        for (off, G, DST) in ((O_NAQ, Gq, NQT), (O_NAK, Gk, NKT)):
            x = xin[n % 2]
            y = xn[n % 2]
            o = xT[n % 2]
            k.dma(x[:], P[i * 128:(i + 1) * 128, off:off + 1024])
            x3 = x[:].rearrange("p (h d) -> p h d", h=8)
            head_rms(k, y[:].rearrange("p (h d) -> p h d", h=8), x3, G[:], ss[:],
                     tmp[:].rearrange("p (h d) -> p h d", h=8), 8, 128)
            ps = k.ps[n % 4].bitcast(BF16) if False else k.ps[n % 4][:].bitcast(BF16)
            for h in range(8):
                k.transpose(ps[:, h * 128:(h + 1) * 128], y[:, h * 128:(h + 1) * 128], ib[:], inc=(h == 7))
            k.copy(o[:], ps.rearrange("p (h t) -> p h t", h=8))
            k.dma(DST[:, :, i * 128:(i + 1) * 128].rearrange("h p t -> p h t"), o[:])
            n += 1
    k.end()


def ph_na_attn(k, I, l, P, NQT, NKT, OT, need_ctx):
    k.begin()
    ib = load_ident_bf(k, I)
    mask = k.sb([64, 64], F32)
    k.dma(mask[:], I["na_mask"][:])
    KT2 = [k.sb([128, T], BF16) for _ in range(2)]
    QT2 = [k.sb([128, T], BF16) for _ in range(2)]
    V642 = [k.sb([64, 68, 129], BF16) for _ in range(2)]
    bias2 = [k.sb([64, 15, 64], F32) for _ in range(2)]
    OTh2 = [k.sb([128, T], BF16) for _ in range(2)]
    Ssb = [k.sb([64, 512], F32) for _ in range(2)]
    PT = [k.sb([64, 768], BF16) for _ in range(3)]
    rs = [k.sb([64, 1], F32) for _ in range(2)]
    On = [k.sb([64, 128], BF16) for _ in range(3)]
    n = 0
    for h in range(8):
        KT, QT, V64, bias, OTh = KT2[h % 2], QT2[h % 2], V642[h % 2], bias2[h % 2], OTh2[h % 2]
        k.dma(KT[:], NKT[h, :, :])
        k.dma(QT[:], NQT[h, :, :])
        k.dma(V64[:, :, 0:128], P[:, O_NAV + h * 128:O_NAV + (h + 1) * 128].rearrange("(r p) d -> p r d", p=64))
        k.memset(V64[:, :, 128:129], 1.0)
        k.dma(bias[:], I["na_bt"][l, h, :, :, :].rearrange("r k q -> k r q"))
        k.tt(bias[:], bias[:], mask[:].rearrange("k (o q) -> k o q", o=1).bc([64, 15, 64]), ALU.add)
        if not need_ctx:
            k.memset(OTh[:, 0:CTX], 0.0)
        blocks = [("lat", r) for r in range(64)] + ([("ctx", r) for r in range(4)] if need_ctx else [])

        def stage_a(kind, r, n_):
            psA = k.ps[n_ % 2]
            psB = k.ps[2 + n_ % 2]
            S = Ssb[n_ % 2]
            pt = PT[n_ % 3]
            if kind == "lat":
                tq = CTX + 64 * r
                r0 = min(max(r - 4, 0), 56)
                dr0 = r0 - r + 7
                for j in range(8):
                    tk = CTX + 64 * (r0 + j)
                    k.matmul(psA[0:64, j * 64:(j + 1) * 64], KT[:, tk:tk + 64], QT[:, tq:tq + 64])
                for j in range(4):
                    k.matmul(psB[0:64, j * 64:(j + 1) * 64], KT[:, 64 * j:64 * j + 64], QT[:, tq:tq + 64])
                k.tt(S[:], psA[0:64, :], bias[:, dr0:dr0 + 8, :].rearrange("k r q -> k (r q)"), ALU.add)
                k.act(pt[:, 0:512], S[:], AF.Exp)
                k.act(pt[:, 512:768], psB[0:64, 0:256], AF.Exp)
            else:
                tq = 64 * r
                for j in range(4):
                    k.matmul(psB[0:64, j * 64:(j + 1) * 64], KT[:, 64 * j:64 * j + 64], QT[:, tq:tq + 64])
                k.act(pt[:, 512:768], psB[0:64, 0:256], AF.Exp)

        def stage_b(kind, r, n_):
            psO = k.ps[4 + n_ % 2]
            pt = PT[n_ % 3]
            if kind == "lat":
                r0 = min(max(r - 4, 0), 56)
                for j in range(8):
                    k.matmul(psO[0:64, 0:129], pt[:, j * 64:(j + 1) * 64], V64[:, 4 + r0 + j, :],
                             start=(j == 0), stop=False)
                for j in range(4):
                    k.matmul(psO[0:64, 0:129], pt[:, 512 + j * 64:512 + (j + 1) * 64], V64[:, j, :],
                             start=False, stop=(j == 3))
            else:
                for j in range(4):
                    k.matmul(psO[0:64, 0:129], pt[:, 512 + j * 64:512 + (j + 1) * 64], V64[:, j, :],
                             start=(j == 0), stop=(j == 3))
            rr = rs[n_ % 2]
            on = On[n_ % 3]
            k.recip(rr[:], psO[0:64, 128:129])
            k.ts(on[:], psO[0:64, 0:128], rr[:, 0:1], ALU.mult)

        def stage_c(kind, r, n_):
            tq = CTX + 64 * r if kind == "lat" else 64 * r
            psT = k.ps[6 + n_ % 2][:].bitcast(BF16)
            on = On[n_ % 3]
            k.transpose(psT[:, 0:64], on[:], ib[0:64, 0:64])
            k.copy(OTh[:, tq:tq + 64], psT[:, 0:64])

        nb = len(blocks)
        for step in range(nb + 2):
            if step < nb:
                stage_a(blocks[step][0], blocks[step][1], n + step)
            if 1 <= step <= nb:
                stage_b(blocks[step - 1][0], blocks[step - 1][1], n + step - 1)
            if 2 <= step:
                stage_c(blocks[step - 2][0], blocks[step - 2][1], n + step - 2)
        n += nb
        k.dma(OT[h, :, :], OTh[:])
    k.end()


def host_consts(inputs):
    c = {}
    q = np.arange(64)
    ws = np.clip(q - 8, 0, 48)
    kk = np.arange(64)
    valid = (kk[:, None] >= ws[None, :]) & (kk[:, None] < ws[None, :] + 16)
    c["na_mask"] = np.where(valid, 0.0, NEG).astype(np.float32)
    dcol = np.clip(kk[:, None] - q[None, :], -15, 15) + 15
    c["na_bt"] = np.ascontiguousarray(inputs["na_rpb"][:, :, :, dcol])
    t = np.arange(SEQ)
    inv = (10000.0 ** (-np.arange(16, dtype=np.float32) / 16)).astype(np.float32)
    C = np.zeros((SEQ, 64), np.float32)
    S = np.zeros((SEQ, 64), np.float32)
    for a, pos in enumerate((t // 64, t % 64)):
        ang = pos.astype(np.float32)[:, None] * inv[None, :]
        C[:, a * 32:a * 32 + 16] = np.cos(ang)
        C[:, a * 32 + 16:a * 32 + 32] = np.cos(ang)
        S[:, a * 32:a * 32 + 16] = -np.sin(ang)
        S[:, a * 32 + 16:a * 32 + 32] = np.sin(ang)
    c["ropeC"] = C
    c["ropeS"] = S
    j = np.arange(64)[:, None]
    i = np.arange(64)[None, :]
    le = [(j <= i), (j >= i)]
    c["gl_L"] = np.stack([np.where(m, -1.0 / 16, 0.0) for m in le]).astype(np.float32)
    c["gl_U"] = np.stack([np.where(~m, -1.0 / 16, 0.0) for m in le]).astype(np.float32)
    c["gl_M"] = np.stack([np.where(m, 1.0, 0.0) for m in le]).astype(np.float32)
    sel = np.zeros((16, 16, 128), np.float32)
    for e in range(16):
        sel[e, e, :] = 1.0
    c["selT"] = sel
    c["ident"] = np.eye(128, dtype=np.float32)
    return c


def mla_scratch(k):
    return dict(CQT=k.dram("CQT", [8, 128, T], BF16), CKVT=k.dram("CKVT", [4, 128, T], BF16),
                Q2=k.dram("Q2", [T, 2304], F32), KV2=k.dram("KV2", [T, 3072], F32),
                MQT=k.dram("MQT", [12, 2, 128, T], BF16), MKT=k.dram("MKT", [12, 2, 128, T], BF16))


def ph_mla(k, I, l, P, S, OT, need_ctx):
    tiles = list(range(NT))
    k.begin()
    ib = load_ident_bf(k, I)
    Gq = k.sb([128, 1024], F32)
    Gk = k.sb([128, 512], F32)
    k.dma(Gq[:], I["mla_qa_norm"][l, :].pbc(128))
    k.dma(Gk[:], I["mla_kva_norm"][l, :].pbc(128))
    xin = [k.sb([128, 1536], F32) for _ in range(2)]
    tmp = k.sb([128, 1024], F32)
    y = [k.sb([128, 1536], BF16) for _ in range(2)]
    ss = k.sb([128, 2], F32)
    oT = [k.sb([128, 12, 128], BF16) for _ in range(2)]
    for n, i in enumerate(tiles):
        x = xin[n % 2]
        yy = y[n % 2]
        o = oT[n % 2]
        k.dma(x[:], P[i * 128:(i + 1) * 128, O_CQ:O_CQ + 1536])
        for (a, w, G, sidx) in ((0, 1024, Gq, 0), (1024, 512, Gk, 1)):
            k.memset(ss[:, sidx:sidx + 1], 0.0)
            k.act(tmp[:, 0:w], x[:, a:a + w], AF.Square, accum=ss[:, sidx:sidx + 1])
            k.rstd(ss[:, sidx:sidx + 1], ss[:, sidx:sidx + 1], w)
            k.stt(yy[:, a:a + w], x[:, a:a + w], ss[:, sidx:sidx + 1], G[:], ALU.mult, ALU.mult)
        for g in range(2):
            ps = k.ps[(2 * n + g) % 4][:].bitcast(BF16)
            cn = 8 if g == 0 else 4
            for c in range(cn):
                cc = g * 8 + c
                k.transpose(ps[:, c * 128:(c + 1) * 128], yy[:, cc * 128:(cc + 1) * 128], ib[:], inc=(c == cn - 1))
            k.copy(o[:, g * 8:g * 8 + cn, :], ps[:, 0:cn * 128].rearrange("p (c t) -> p c t", c=cn))
        k.dma(S["CQT"][:, :, i * 128:(i + 1) * 128].rearrange("c p t -> p c t"), o[:, 0:8, :])
        k.dma(S["CKVT"][:, :, i * 128:(i + 1) * 128].rearrange("c p t -> p c t"), o[:, 8:12, :])
    k.end()

    def setup():
        return [k.sb([128, 512], F32) for _ in range(3)]

    def mk_ep(DST):
        def ep(ob, i, n0, nn, ps):
            o = ob[(i + n0 // 512) % 3]
            k.copy(o[:, 0:nn], ps)
            k.dma(DST[i * 128:(i + 1) * 128, n0:n0 + nn], o[:, 0:nn])
        return ep
    gemm(k, S["CQT"], 8, I["mla_w_uq"][l], 2304, tiles, mk_ep(S["Q2"]), slab=9, setup=setup)
    gemm(k, S["CKVT"], 4, I["mla_w_ukv"][l], 3072, tiles, mk_ep(S["KV2"]), slab=9, setup=setup)

    k.begin()
    ib = load_ident_bf(k, I)
    Gq = k.sb([128, 12, 192], F32)
    Gkn = k.sb([128, 128], F32)
    Gkr = k.sb([128, 64], F32)
    for h in range(12):
        k.dma(Gq[:, h, :], I["mla_q_norm"][l, :].pbc(128))
    k.ts(Gq[:], Gq[:], 192.0 ** -0.5, ALU.mult)
    k.dma(Gkn[:], I["mla_k_norm"][l, 0:128].pbc(128))
    k.dma(Gkr[:], I["mla_k_norm"][l, 128:192].pbc(128))
    qin = [k.sb([128, 12, 192], F32) for _ in range(2)]
    kvin = [k.sb([128, 12, 256], F32) for _ in range(2)]
    krin = [k.sb([128, 64], F32) for _ in range(2)]
    rc = [k.sb([128, 64], F32) for _ in range(2)]
    rsn = [k.sb([128, 64], F32) for _ in range(2)]
    tq = k.sb([128, 12, 192], F32)
    qn = k.sb([128, 12, 192], F32)
    xsw = k.sb([128, 12, 64], F32)
    t1 = k.sb([128, 12, 64], F32)
    qb = k.sb([128, 12, 192], BF16)
    kn = k.sb([128, 12, 192], F32)
    kb = k.sb([128, 12, 192], BF16)
    krg = k.sb([128, 64], F32)
    ss = k.sb([128, 12], F32)
    skr = k.sb([128, 1], F32)
    oa = [k.sb([128, 12, 128], BF16) for _ in range(2)]
    ob_ = [k.sb([64, 12, 128], BF16) for _ in range(2)]

    def rope(x3, nh, n):
        x5 = x3.rearrange("p h (a b d) -> p h a b d", a=2, b=2)
        xs3 = xsw[:, 0:nh, :]
        xs5 = xs3.rearrange("p h (a b d) -> p h a b d", a=2, b=2)
        for a in range(2):
            k.copy(xs5[:, :, a, 0, :], x5[:, :, a, 1, :], eng="dve")
            k.copy(xs5[:, :, a, 1, :], x5[:, :, a, 0, :], eng="dve")
        cb = rc[n % 2][:].rearrange("p (o d) -> p o d", o=1).bc([128, nh, 64])
        sb_ = rsn[n % 2][:].rearrange("p (o d) -> p o d", o=1).bc([128, nh, 64])
        k.tt(t1[:, 0:nh, :], x3, cb, ALU.mult)
        k.tt(xs3, xs3, sb_, ALU.mult)
        k.tt(x3, t1[:, 0:nh, :], xs3, ALU.add)

    cnt = 0
    for n, i in enumerate(tiles):
        latent = i >= 2
        q = qin[n % 2]
        kv = kvin[n % 2]
        kr = krin[n % 2]
        k.dma(q[:].rearrange("p h d -> p (h d)"), S["Q2"][i * 128:(i + 1) * 128, :])
        k.dma(kv[:].rearrange("p h d -> p (h d)"), S["KV2"][i * 128:(i + 1) * 128, :])
        k.dma(kr[:], P[i * 128:(i + 1) * 128, O_KR:O_KR + 64])
        if latent:
            k.dma(rc[n % 2][:], I["ropeC"][(i - 2) * 128:(i - 1) * 128, :])
            k.dma(rsn[n % 2][:], I["ropeS"][(i - 2) * 128:(i - 1) * 128, :])
        head_rms(k, qn[:], q[:], Gq[:], ss[:], tq[:], 12, 192)
        if latent:
            rope(qn[:, :, 128:192], 12, n)
        k.copy(qb[:], qn[:], eng="act")
        k.memset(skr[:], 0.0)
        k.act(t1[:, 0, :], kr[:], AF.Square, accum=skr[:])
        head_rms(k, kn[:, :, 0:128], kv[:, :, 0:128], Gkn[:].rearrange("p (o d) -> p o d", o=1).bc([128, 12, 128]),
                 ss[:], tq[:, :, 0:128], 12, 128, extra=(skr[:, 0:1], 192))
        k.tt(krg[:], kr[:], Gkr[:], ALU.mult)
        if latent:
            rope(krg[:].rearrange("p (o d) -> p o d", o=1), 1, n)
        k.tt(kn[:, :, 128:192], krg[:].rearrange("p (o d) -> p o d", o=1).bc([128, 12, 64]),
             ss[:].rearrange("p (h o) -> p h o", o=1).bc([128, 12, 64]), ALU.mult)
        k.copy(kb[:], kn[:], eng="act")
        for (src, DST) in ((qb, S["MQT"]), (kb, S["MKT"])):
            a = oa[cnt % 2]
            b = ob_[cnt % 2]
            cnt += 1
            for g in range(2):
                hs = range(8) if g == 0 else range(8, 12)
                ps = k.ps[(2 * cnt + g) % 4][:].bitcast(BF16)
                for jj, h in enumerate(hs):
                    k.transpose(ps[:, jj * 128:(jj + 1) * 128], src[:, h, 0:128], ib[:], inc=(jj == len(hs) - 1))
                k.copy(a[:, hs[0]:hs[0] + len(hs), :], ps[:, 0:len(hs) * 128].rearrange("p (c t) -> p c t", c=len(hs)))
            for g in range(2):
                hs = range(8) if g == 0 else range(8, 12)
                ps = k.ps[4 + (2 * cnt + g) % 4][:].bitcast(BF16)
                for jj, h in enumerate(hs):
                    k.transpose(ps[0:64, jj * 128:(jj + 1) * 128], src[:, h, 128:192], ib[:], inc=(jj == len(hs) - 1))
                k.copy(b[:, hs[0]:hs[0] + len(hs), :], ps[0:64, 0:len(hs) * 128].rearrange("p (c t) -> p c t", c=len(hs)))
            k.dma(DST[:, 0, :, i * 128:(i + 1) * 128].rearrange("h p t -> p h t"), a[:])
            k.dma(DST[:, 1, 0:64, i * 128:(i + 1) * 128].rearrange("h p t -> p h t"), b[:])
    k.end()

    k.begin()
    KTa2 = [k.sb([128, T], BF16) for _ in range(2)]
    KTb2 = [k.sb([64, T], BF16) for _ in range(2)]
    QTa2 = [k.sb([128, T], BF16) for _ in range(2)]
    QTb2 = [k.sb([64, T], BF16) for _ in range(2)]
    Vt2 = [k.sb([128, NT, 128], BF16) for _ in range(2)]
    OTh2 = [k.sb([128, T], BF16) for _ in range(2)]
    ones = k.sb([128, 128], BF16)
    k.memset(ones[:], 1.0)
    PT = [k.sb([128, 512], BF16) for _ in range(4)]
    rec = [k.sb([128, 512], F32) for _ in range(2)]
    n = 0
    m = 0
    for h in range(12):
        KTa, KTb, QTa, QTb, Vt, OTh = KTa2[h % 2], KTb2[h % 2], QTa2[h % 2], QTb2[h % 2], Vt2[h % 2], OTh2[h % 2]
        k.dma(KTa[:], S["MKT"][h, 0, :, :])
        k.dma(KTb[:], S["MKT"][h, 1, 0:64, :])
        k.dma(QTa[:], S["MQT"][h, 0, :, :])
        k.dma(QTb[:], S["MQT"][h, 1, 0:64, :])
        k.dma(Vt[:], S["KV2"][:, h * 256 + 128:h * 256 + 256].rearrange("(t p) d -> p t d", p=128))
        if not need_ctx:
            k.memset(OTh[:, 0:CTX], 0.0)
        blocks = [(CTX + 512 * qb_, 512, NT) for qb_ in range(8)] + ([(0, 256, 2)] if need_ctx else [])
        for (q0, nq, nkt) in blocks:
            def score(kt, n_):
                Sp = k.ps[(0, 1, 6)[n_ % 3]]
                pt = PT[n_ % 4]
                k.matmul(Sp[:, 0:nq], KTa[:, kt * 128:(kt + 1) * 128], QTa[:, q0:q0 + nq], start=True, stop=False)
                k.matmul(Sp[:, 0:nq], KTb[:, kt * 128:(kt + 1) * 128], QTb[:, q0:q0 + nq], start=False, stop=True)
                k.act(pt[:, 0:nq], Sp[:, 0:nq], AF.Exp)

            po = k.ps[2 + 2 * (m % 2)]
            psm = k.ps[3 + 2 * (m % 2)]
            score(0, n)
            if nkt > 1:
                score(1, n + 1)
            for kt in range(nkt):
                if kt + 2 < nkt:
                    score(kt + 2, n + 2)
                pt = PT[n % 4]
                n += 1
                k.matmul(po[:, 0:nq], Vt[:, kt, :], pt[:, 0:nq], start=(kt == 0), stop=(kt == nkt - 1))
                k.matmul(psm[:, 0:nq], ones[:], pt[:, 0:nq], start=(kt == 0), stop=(kt == nkt - 1))
            rc_ = rec[m % 2]
            m += 1
            k.recip(rc_[:, 0:nq], psm[:, 0:nq])
            k.tt(OTh[:, q0:q0 + nq], po[:, 0:nq], rc_[:, 0:nq], ALU.mult)
        k.dma(OT[8 + h, :, :], OTh[:])
    k.end()


def gla_scratch(k):
    return dict(OF=k.dram("OF", [T, 1536], F32), OB=k.dram("OB", [T, 1536], F32))


def ph_gla(k, I, l, P, S, OT):
    k.begin()
    identf = k.sb([128, 128], F32)
    k.dma(identf[:], I["ident"][:])
    ib = load_ident_bf(k, I)
    Lm = [k.sb([64, 64], F32) for _ in range(2)]
    Um = [k.sb([64, 64], F32) for _ in range(2)]
    Mk = [k.sb([64, 64], F32) for _ in range(2)]
    w2 = [k.sb([17, 768], F32) for _ in range(2)]
    for d_ in range(2):
        k.dma(Lm[d_][:], I["gl_L"][d_, :, :])
        k.dma(Um[d_][:], I["gl_U"][d_, :, :])
        k.dma(Mk[d_][:], I["gl_M"][d_, :, :])
        nm = "f" if d_ == 0 else "b"
        k.dma(w2[d_][0:16, :], I["gla_w_gate_" + nm][l, :, :])
        k.dma(w2[d_][16:17, :], I["gla_b_gate_" + nm][l:l + 1, :])
    negs = k.sb([64, 1], F32)
    k.memset(negs[:], -1.0 / 16)
    NB = 3
    aT = [k.sb([17, 64], F32) for _ in range(NB)]
    for t_ in aT:
        k.memset(t_[:], 1.0)
    qin = [k.sb([64, 768], F32) for _ in range(NB)]
    kin = [k.sb([64, 768], F32) for _ in range(NB)]
    vin = [k.sb([64, 1536], BF16) for _ in range(NB)]
    ain = [k.sb([64, 16], F32) for _ in range(NB)]
    ez = [k.sb([64, 768], F32) for _ in range(2)]
    l6 = [k.sb([64, 768], F32) for _ in range(NB)]
    E1 = [k.sb([64, 768], F32) for _ in range(2)]
    E2 = [k.sb([64, 768], F32) for _ in range(2)]
    E3 = [k.sb([64, 768], F32) for _ in range(2)]
    eB = [k.sb([128, 6], F32) for _ in range(2)]
    qe = [k.sb([64, 768], BF16) for _ in range(2)]
    ke = [k.sb([64, 768], BF16) for _ in range(2)]
    kend = [k.sb([64, 768], BF16) for _ in range(2)]
    qkT = [k.sb([128, 128], BF16) for _ in range(6)]
    attT = [k.sb([64, 64], BF16) for _ in range(6)]
    osb = [k.sb([64, 1536], F32) for _ in range(NB)]
    Sf = [[k.sb([128, 256], F32) for _ in range(6)] for _ in range(2)]
    Sb = [[k.sb([128, 256], BF16) for _ in range(6)] for _ in range(2)]
    for d_ in range(2):
        for h in range(6):
            k.memset(Sf[d_][h][:], 0.0)
            k.memset(Sb[d_][h][:], 0.0)
    orders = [list(range(68)), [3, 2, 1, 0] + list(range(67, 3, -1))]
    nn = 0
    cc = 0
    for ci in range(68):
        for d_ in range(2):
            DST = S["OF"] if d_ == 0 else S["OB"]
            aoff = O_AF if d_ == 0 else O_AB
            c = orders[d_][ci]
            t0 = c * 64
            sl = cc % NB
            s2 = cc % 2
            cc += 1
            q, kk, v, a, at, lg, o = qin[sl], kin[sl], vin[sl], ain[sl], aT[sl], l6[sl], osb[sl]
            ezz = ez[s2]
            e1, e2, e3, eb = E1[s2], E2[s2], E3[s2], eB[s2]
            qe_, ke_, kd_ = qe[s2], ke[s2], kend[s2]
            k.dma(q[:], P[t0:t0 + 64, O_GQ:O_GQ + 768])
            k.dma(kk[:], P[t0:t0 + 64, O_GK:O_GK + 768])
            k.dma(v[:], P[t0:t0 + 64, O_GV:O_GV + 1536])
            k.dma(a[:], P[t0:t0 + 64, aoff:aoff + 16])
            pz = (k.ps[0], k.ps[1])
            pB = (k.ps[2], k.ps[3])
            k.transpose(pz[0][0:16, 0:64], a[:], identf[0:64, 0:64])
            k.copy(at[0:16, :], pz[0][0:16, 0:64], eng="dve")
            for hh in range(2):
                k.matmul(pz[hh][0:64, 0:384], at[:], w2[d_][:, hh * 384:(hh + 1) * 384])
            for hh in range(2):
                k.act(ezz[:, hh * 384:(hh + 1) * 384], pz[hh][0:64, 0:384], AF.Exp, scale=-1.0)
            k.act(lg[:], ezz[:], AF.Ln, bias=1.0)
            for hh in range(2):
                k.matmul(pB[hh][0:64, 0:384], Lm[d_][:], lg[:, hh * 384:(hh + 1) * 384])
            for hh in range(2):
                k.matmul(pz[hh][0:64, 0:384], Um[d_][:], lg[:, hh * 384:(hh + 1) * 384])
            for h in range(6):
                k.matmul(pB[1][:, 384 + h:385 + h], lg[:, h * 128:(h + 1) * 128], negs[:])
            for hh in range(2):
                k.act(e1[:, hh * 384:(hh + 1) * 384], pB[hh][0:64, 0:384], AF.Exp)
                k.act(e2[:, hh * 384:(hh + 1) * 384], pB[hh][0:64, 0:384], AF.Exp, scale=-1.0)
                k.act(e3[:, hh * 384:(hh + 1) * 384], pz[hh][0:64, 0:384], AF.Exp)
            k.act(eb[:], pB[1][:, 384:390], AF.Exp)
            k.stt(qe_[:], q[:], 128.0 ** -0.5, e1[:], ALU.mult, ALU.mult)
            k.tt(ke_[:], kk[:], e2[:], ALU.mult)
            k.tt(kd_[:], kk[:], e3[:], ALU.mult)
            b4, b5, b6, b7 = k.ps[4], k.ps[5], k.ps[6], k.ps[7]
            outv = (b5[0:64, 0:256], b5[0:64, 256:512], b6[0:64, 0:256])
            uv = (b6[:, 256:512], b7[:, 0:256], b7[:, 256:512])
            for g in range(2):
                hs_ = [3 * g + j for j in range(3)]
                ptp = b4[:, 0:192].bitcast(BF16)
                qks = [qkT[(nn + j) % 6] for j in range(3)]
                ats = [attT[(nn + j) % 6] for j in range(3)]
                nn += 3
                for j, h in enumerate(hs_):
                    hs = slice(h * 128, (h + 1) * 128)
                    k.transpose(ptp[:, j * 128:j * 128 + 64], qe_[:, hs], ib[0:64, 0:64], inc=False)
                    k.transpose(ptp[:, j * 128 + 64:(j + 1) * 128], ke_[:, hs], ib[0:64, 0:64], inc=(j == 2))
                for j, h in enumerate(hs_):
                    k.copy(qks[j][:], ptp[:, j * 128:(j + 1) * 128], eng="dve")
                for j, h in enumerate(hs_):
                    k.matmul(b4[0:64, 256 + j * 64:256 + (j + 1) * 64], qks[j][:, 64:128], qks[j][:, 0:64])
                for j, h in enumerate(hs_):
                    k.tt(ats[j][:], b4[0:64, 256 + j * 64:256 + (j + 1) * 64], Mk[d_][:], ALU.mult)
                for j, h in enumerate(hs_):
                    k.matmul(outv[j], qks[j][:, 0:64], Sb[d_][h][:], start=True, stop=False)
                    k.matmul(outv[j], ats[j][:], v[:, h * 256:(h + 1) * 256], start=False, stop=True)
                for j, h in enumerate(hs_):
                    k.copy(o[:, h * 256:(h + 1) * 256], outv[j], eng="act")
                    k.matmul(uv[j], kd_[:, h * 128:(h + 1) * 128], v[:, h * 256:(h + 1) * 256])
                for j, h in enumerate(hs_):
                    k.stt(Sf[d_][h][:], Sf[d_][h][:], eb[:, h:h + 1], uv[j], ALU.mult, ALU.add)
                    k.copy(Sb[d_][h][:], Sf[d_][h][:], eng="dve")
            k.dma(DST[t0:t0 + 64, :], o[:])
    k.end()

    k.begin()
    ib = load_ident_bf(k, I)
    G = k.sb([128, 6, 256], F32)
    for h in range(6):
        k.dma(G[:, h, :], I["gla_out_norm"][l, :].pbc(128))
    of = [k.sb([128, 1536], F32) for _ in range(2)]
    ob = [k.sb([128, 1536], F32) for _ in range(2)]
    gg = [k.sb([128, 1536], F32) for _ in range(2)]
    tmp = k.sb([128, 1536], F32)
    on = k.sb([128, 1536], F32)
    yb = [k.sb([128, 1536], BF16) for _ in range(2)]
    ss = k.sb([128, 6], F32)
    oT = [k.sb([128, 12, 128], BF16) for _ in range(2)]
    for n, i in enumerate(range(NT)):
        a, b, g, y, o = of[n % 2], ob[n % 2], gg[n % 2], yb[n % 2], oT[n % 2]
        k.dma(a[:], S["OF"][i * 128:(i + 1) * 128, :])
        k.dma(b[:], S["OB"][i * 128:(i + 1) * 128, :])
        k.dma(g[:], P[i * 128:(i + 1) * 128, O_GG:O_GG + 1536])
        k.tt(a[:], a[:], b[:], ALU.add)
        head_rms(k, on[:].rearrange("p (h d) -> p h d", h=6), a[:].rearrange("p (h d) -> p h d", h=6), G[:], ss[:],
                 tmp[:].rearrange("p (h d) -> p h d", h=6), 6, 256)
        k.act(g[:], g[:], AF.Silu)
        k.tt(y[:], on[:], g[:], ALU.mult)
        for gi in range(2):
            ps = k.ps[(2 * n + gi) % 4][:].bitcast(BF16)
            cn = 8 if gi == 0 else 4
            for c in range(cn):
                cc = gi * 8 + c
                k.transpose(ps[:, c * 128:(c + 1) * 128], y[:, cc * 128:(cc + 1) * 128], ib[:], inc=(c == cn - 1))
            k.copy(o[:, gi * 8:gi * 8 + cn, :], ps[:, 0:cn * 128].rearrange("p (c t) -> p c t", c=cn))
        k.dma(OT[20:32, :, i * 128:(i + 1) * 128].rearrange("c p t -> p c t"), o[:])
    k.end()


def resid_gemm(k, AT, Kc, W, MOD, l, gi, Xin, Xout, tiles, slab=4):
    rs_needed = sorted({1 if i < 2 else 0 for i in tiles})

    def setup():
        gb = {}
        for r in rs_needed:
            gb[r] = k.sb([128, D], F32)
            k.dma(gb[r][:], MOD[l, r, gi * D:(gi + 1) * D].pbc(128))
        return dict(gb=gb, xo=[k.sb([128, 512], F32) for _ in range(3)], xn=[k.sb([128, 512], F32) for _ in range(3)],
                    n=0)

    def ep(c, i, n0, nn, ps):
        r = 1 if i < 2 else 0
        xo = c["xo"][c["n"] % 3]
        xn = c["xn"][c["n"] % 3]
        c["n"] += 1
        k.dma(xo[:, 0:nn], Xin[i * 128:(i + 1) * 128, n0:n0 + nn])
        k.tt(xn[:, 0:nn], ps, c["gb"][r][:, n0:n0 + nn], ALU.mult)
        k.tt(xn[:, 0:nn], xn[:, 0:nn], xo[:, 0:nn], ALU.add)
        k.dma(Xout[i * 128:(i + 1) * 128, n0:n0 + nn], xn[:, 0:nn])

    gemm(k, AT, Kc, W, D, tiles, ep, slab=slab, setup=setup)


def ph_moe_up(k, I, l, HT, GATET, GP, tiles, slab=9):
    k.begin()
    at = k.sb([128, 32, slab * 128], BF16)
    gts = k.sb([16, slab * 128], F32)
    selT = k.sb([16, 16, 128], F32)
    k.dma(selT[:], I["selT"][:].rearrange("e k m -> k e m"))
    GB = k.sb([128, slab * 128], F32)
    w1g = [k.sb([128, 32, 256], BF16) for _ in range(2)]
    w3g = [k.sb([128, 32, 256], BF16) for _ in range(2)]
    sa = [k.sb([128, 512], F32) for _ in range(2)]
    tm = [k.sb([128, 512], F32) for _ in range(2)]
    go = [k.sb([128, 512], BF16) for _ in range(3)]
    nw = 0
    nx = 0
    for s0 in range(0, len(tiles), slab):
        st = tiles[s0:s0 + slab]
        tok0 = st[0] * 128
        ntok = len(st) * 128
        groups = [(o_, min(512, ntok - o_)) for o_ in range(0, ntok, 512)]
        k.dma(at[:, :, 0:ntok], HT[:, :, tok0:tok0 + ntok].rearrange("c p t -> p c t"))
        k.dma(gts[:, 0:ntok], GATET[:, tok0:tok0 + ntok])
        for e in range(16):
            for (o_, n_) in groups:
                k.matmul(k.ps[0][:, 0:n_], selT[:, e, :], gts[:, o_:o_ + n_])
                k.copy(GB[:, o_:o_ + n_], k.ps[0][:, 0:n_], eng="dve")
            for fg in range(4):
                w1 = w1g[nw % 2]
                w3 = w3g[nw % 2]
                nw += 1
                for (w, nm) in ((w1, "moe_w1"), (w3, "moe_w3")):
                    for c0 in (0, 16):
                        k.dma(w[:, c0:c0 + 16, :],
                              I[nm][l, e, c0 * 128:(c0 + 16) * 128, fg * 256:(fg + 1) * 256].rearrange("(c p) n -> p c n", p=128))
                for f2 in range(2):
                    for (o_, n_) in groups:
                        pa = k.ps[2 + nx % 2]
                        pb = k.ps[4 + nx % 2]
                        s_ = sa[nx % 2]
                        t_ = tm[nx % 2]
                        g_ = go[nx % 3]
                        nx += 1
                        for c in range(32):
                            k.matmul(pa[:, 0:n_], w1[:, c, f2 * 128:(f2 + 1) * 128], at[:, c, o_:o_ + n_],
                                     start=(c == 0), stop=(c == 31))
                        for c in range(32):
                            k.matmul(pb[:, 0:n_], w3[:, c, f2 * 128:(f2 + 1) * 128], at[:, c, o_:o_ + n_],
                                     start=(c == 0), stop=(c == 31))
                        k.act(s_[:, 0:n_], pa[:, 0:n_], AF.Silu)
                        k.tt(t_[:, 0:n_], s_[:, 0:n_], pb[:, 0:n_], ALU.mult)
                        k.tt(g_[:, 0:n_], t_[:, 0:n_], GB[:, o_:o_ + n_], ALU.mult)
                        k.dma(GP[e * 8 + fg * 2 + f2, :, tok0 + o_:tok0 + o_ + n_], g_[:, 0:n_])
    k.end()


def ph_moe_down(k, I, l, GP, MOD, Xa, Xb, tiles):
    W2 = I["moe_w2"][l].rearrange("e f n -> (e f) n")
    src, dst = Xa, Xb
    for q in range(4):
        resid_gemm(k, GP[q * 32:(q + 1) * 32], 32, W2[q * 4096:(q + 1) * 4096, :], MOD, l, 5, src, dst, tiles)
        src, dst = dst, src
    return src


INPUT_SHAPES = None


def build_program(shapes):
    k = K()
    I = {}
    for nm, shp in shapes.items():
        I[nm] = k.dram(nm, list(shp), F32, kind="ExternalInput")
    OUT = k.dram("out", [SEQ, D], F32, kind="ExternalOutput")
    XA = k.dram("XA", [T, D], F32)
    XB = k.dram("XB", [T, D], F32)
    MOD = k.dram("MOD", [DEPTH, 2, 6 * D], F32)
    HT = k.dram("HT", [32, 128, T], BF16)
    P = k.dram("P", [T, IN_W], F32)
    OT = k.dram("OT", [32, 128, T], BF16)
    NQT = k.dram("NQT", [8, 128, T], BF16)
    NKT = k.dram("NKT", [8, 128, T], BF16)
    GATE = k.dram("GATE", [T, 16], F32)
    GATET = k.dram("GATET", [16, T], F32)
    GP = k.dram("GP", [128, 128, T], BF16)
    MS = mla_scratch(k)
    GS = gla_scratch(k)
    k.dma(XA[0:CTX, :], I["ctx"][:], q="sp")
    for j in range(4):
        k.dma(XA[CTX + j * 1024:CTX + (j + 1) * 1024, :], I["x"][j * 1024:(j + 1) * 1024, :], q="sp")
    ph_ada(k, I, MOD)
    Xc, Xo = XA, XB
    allt = list(range(NT))
    latt = list(range(2, NT))
    for l in range(DEPTH):
        need_ctx = l < DEPTH - 1
        tl = allt if need_ctx else latt
        ph_norm(k, I, l, 0, Xc, MOD, HT, allt)
        ph_proj_in(k, I, l, HT, P, allt)
        ph_na_prep(k, I, l, P, NQT, NKT, allt)
        ph_na_attn(k, I, l, P, NQT, NKT, OT, need_ctx)
        ph_mla(k, I, l, P, MS, OT, need_ctx)
        ph_gla(k, I, l, P, GS, OT)
        resid_gemm(k, OT, 32, I["w_out"][l], MOD, l, 2, Xc, Xo, tl)
        ph_norm(k, I, l, 1, Xo, MOD, HT, tl, router=dict(GATE=GATE, GATET=GATET))
        ph_moe_up(k, I, l, HT, GATET, GP, tl, slab=(9 if need_ctx else 8))
        fin = ph_moe_down(k, I, l, GP, MOD, Xo, Xc, tl)
        Xc, Xo = (fin, Xc if fin is Xo else Xo)
    for j in range(4):
        k.dma(OUT[j * 1024:(j + 1) * 1024, :], Xc[CTX + j * 1024:CTX + (j + 1) * 1024, :], q="sp")
    k.barrier()
    return k


def kernel(**inputs):
    inp = {n: np.asarray(v) for n, v in inputs.items()}
    B = inp["x"].shape[0]
    consts = host_consts(inp)
    shared = {}
    for nm in ("w_ada", "b_ada", "norm1", "norm2", "w_in", "w_out", "na_q_norm", "na_k_norm", "mla_qa_norm",
               "mla_kva_norm", "mla_w_uq", "mla_w_ukv", "mla_q_norm", "mla_k_norm", "gla_w_gate_f", "gla_b_gate_f",
               "gla_w_gate_b", "gla_b_gate_b", "gla_out_norm", "router_bias", "moe_w1", "moe_w3", "moe_w2"):
        shared[nm] = np.ascontiguousarray(inp[nm], dtype=np.float32)
    shared["wrT"] = np.ascontiguousarray(inp["w_router"].reshape(32, 128, 16).transpose(1, 0, 2), dtype=np.float32)
    for nm, v in consts.items():
        shared[nm] = np.ascontiguousarray(v, dtype=np.float32)
    in_maps = []
    for b in range(B):
        m = dict(shared)
        m["x"] = np.ascontiguousarray(inp["x"][b], dtype=np.float32)
        m["ctx"] = np.ascontiguousarray(inp["ctx"][b], dtype=np.float32)
        cvec = np.stack([inp["c"][b], inp["c_ctx"]], 0).astype(np.float32)
        m["cT"] = np.ascontiguousarray(cvec.reshape(2, 32, 128).transpose(2, 1, 0))
        in_maps.append(m)
    shapes = {nm: v.shape for nm, v in in_maps[0].items()}
    k = build_program(shapes)
    res = run_bass_kernel_spmd(k.nc, in_maps, core_ids=list(range(B)))
    return np.stack([res.results[b]["out"] for b in range(B)], 0).astype(np.float32)
```
